# Optimizing a Trainium2 kernel written in Bass

```python
import math
import jax, jax.numpy as jnp
from jax import lax
import numpy as np

D_MODEL = 1024
BATCH = 8
SEQ = 2048
DEPTH = 2

N_A_LAYERS = DEPTH // 2
N_B_LAYERS = DEPTH - N_A_LAYERS
N_DENSE_FFN = (DEPTH + 1) // 2
N_MOE_FFN = DEPTH // 2
RMS_EPS = 1e-6
ROPE_THETA = 500000.0
NEG_INF = -1e30

MLA_HEADS = 16
MLA_NOPE_DIM = 64
MLA_ROPE_DIM = 32
MLA_V_DIM = 64
MLA_Q_LORA = 384
MLA_KV_LORA = 256
MLA_QK_DIM = MLA_NOPE_DIM + MLA_ROPE_DIM
MLA_DOWN_DIM = MLA_Q_LORA + MLA_KV_LORA + MLA_ROPE_DIM
Q_BLOCK = 128

DSW_BRANCHES = ((128, 1), (512, 4), (2048, 16))
N_BR = len(DSW_BRANCHES)
DSW_HEADS = 16
DSW_HEAD_DIM = 64
DSW_ROT_DIM = DSW_HEAD_DIM // 4
DSW_WIDTH = DSW_HEADS * DSW_HEAD_DIM
DSW_BLOCK = 128

FFN_DIM = 2816
N_EXPERTS = 8
TOP_K = 2
EXPERT_DIM = 3584

kernel_name = "yoco_mla_dilated_moe_trunk"


def rmsnorm(x, g):
    x32 = x.astype(jnp.float32)
    y = x32 * lax.rsqrt(jnp.mean(x32 * x32, axis=-1, keepdims=True) + RMS_EPS)
    return (y * g.astype(jnp.float32)).astype(x.dtype)


def rope(t, pos, rot_dim):
    half = rot_dim // 2
    inv_freq = jnp.power(jnp.float32(ROPE_THETA), -jnp.arange(half, dtype=jnp.float32) * (2.0 / rot_dim))
    ang = pos.astype(jnp.float32)[..., None] * inv_freq
    cos = jnp.cos(ang)[:, :, None, :]
    sin = jnp.sin(ang)[:, :, None, :]
    x1 = t[..., :half].astype(jnp.float32)
    x2 = t[..., half:rot_dim].astype(jnp.float32)
    rot = jnp.concatenate([x1 * cos - x2 * sin, x2 * cos + x1 * sin], axis=-1).astype(t.dtype)
    return jnp.concatenate([rot, t[..., rot_dim:]], axis=-1)


def blocked_causal_attention(q, k, v, scale):
    B, S, H, dq = q.shape
    dv = v.shape[-1]
    nb = S // Q_BLOCK
    qb = q.reshape(B, nb, Q_BLOCK, H, dq).transpose(1, 0, 2, 3, 4)
    kpos = jnp.arange(S)

    def one_block(args):
        q_blk, i = args
        s = jnp.einsum('bqhd,bkhd->bhqk', q_blk, k).astype(jnp.float32) * scale
        qpos = i * Q_BLOCK + jnp.arange(Q_BLOCK)
        s = jnp.where(kpos[None, :] <= qpos[:, None], s, NEG_INF)
        p = jax.nn.softmax(s, axis=-1).astype(v.dtype)
        return jnp.einsum('bhqk,bkhd->bqhd', p, v)

    o = lax.map(one_block, (qb, jnp.arange(nb)))
    return o.transpose(1, 0, 2, 3, 4).reshape(B, S, H, dv)


def mla_attention(xn, pos, w_down, q_norm, w_uq, kv_norm, w_ukv, w_o):
    B, S, _ = xn.shape
    down = xn @ w_down
    c_q = rmsnorm(down[..., :MLA_Q_LORA], q_norm)
    c_kv = rmsnorm(down[..., MLA_Q_LORA:MLA_Q_LORA + MLA_KV_LORA], kv_norm)
    k_rope = rope(down[..., None, MLA_Q_LORA + MLA_KV_LORA:], pos, MLA_ROPE_DIM)
    q = (c_q @ w_uq).reshape(B, S, MLA_HEADS, MLA_QK_DIM)
    q = jnp.concatenate([q[..., :MLA_NOPE_DIM], rope(q[..., MLA_NOPE_DIM:], pos, MLA_ROPE_DIM)], axis=-1)
    kv = (c_kv @ w_ukv).reshape(B, S, MLA_HEADS, MLA_NOPE_DIM + MLA_V_DIM)
    k = jnp.concatenate([kv[..., :MLA_NOPE_DIM],
                         jnp.broadcast_to(k_rope, (B, S, MLA_HEADS, MLA_ROPE_DIM))], axis=-1)
    v = kv[..., MLA_NOPE_DIM:]
    o = blocked_causal_attention(q, k, v, 1.0 / math.sqrt(MLA_QK_DIM))
    return o.reshape(B, S, MLA_HEADS * MLA_V_DIM) @ w_o


def shared_dsw_kv(h, pos, kv_norm, w_kv):
    B, S, _ = h.shape
    kv = (rmsnorm(h, kv_norm) @ w_kv).reshape(B, S, 2, N_BR, DSW_HEADS, DSW_HEAD_DIM)
    k = rope(kv[:, :, 0].reshape(B, S, N_BR * DSW_HEADS, DSW_HEAD_DIM), pos, DSW_ROT_DIM)
    return k.reshape(B, S, N_BR, DSW_HEADS, DSW_HEAD_DIM), kv[:, :, 1]


def dilated_branch(q, k, v, window, dilation):
    B, S, H, hd = q.shape
    L = S // dilation
    w_sub = window // dilation
    nb = -(-L // DSW_BLOCK)
    Lp = nb * DSW_BLOCK

    def to_sub(t):
        return t.reshape(B, L, dilation, H, hd).transpose(0, 2, 1, 3, 4)

    qs = jnp.pad(to_sub(q), ((0, 0), (0, 0), (0, Lp - L), (0, 0), (0, 0)))
    pad_kv = ((0, 0), (0, 0), (DSW_BLOCK, Lp - L), (0, 0), (0, 0))
    ks = jnp.pad(to_sub(k), pad_kv).reshape(B, dilation, nb + 1, DSW_BLOCK, H, hd)
    vs = jnp.pad(to_sub(v), pad_kv).reshape(B, dilation, nb + 1, DSW_BLOCK, H, hd)
    k_win = jnp.concatenate([ks[:, :, :-1], ks[:, :, 1:]], axis=3)
    v_win = jnp.concatenate([vs[:, :, :-1], vs[:, :, 1:]], axis=3)
    qb = qs.reshape(B, dilation, nb, DSW_BLOCK, H, hd)

    s = jnp.einsum('brnqhd,brnkhd->brnhqk', qb, k_win).astype(jnp.float32) * (1.0 / math.sqrt(hd))
    a = jnp.arange(DSW_BLOCK)[:, None]
    b = jnp.arange(2 * DSW_BLOCK)[None, :]
    dist = a + DSW_BLOCK - b
    key_sub = jnp.arange(nb)[:, None, None] * DSW_BLOCK - DSW_BLOCK + b[None]
    valid = (dist >= 0)[None] & (dist <= w_sub)[None] & (key_sub >= 0)
    s = jnp.where(valid[None, None, :, None], s, NEG_INF)
    lse = jax.nn.logsumexp(s, axis=-1)
    p = jnp.exp(s - lse[..., None]).astype(v.dtype)
    o = jnp.einsum('brnhqk,brnkhd->brnqhd', p, v_win)

    def from_sub(t):
        t = t.reshape(B, dilation, Lp, H, t.shape[-1])[:, :, :L]
        return t.transpose(0, 2, 1, 3, 4).reshape(B, S, H, t.shape[-1])

    o = from_sub(o)
    lse = from_sub(lse.transpose(0, 1, 2, 4, 3)[..., None])[..., 0]
    return o, lse


def dsw_attention(xn, pos, k_sh, v_sh, w_q, w_o):
    B, S, _ = xn.shape
    q = (xn @ w_q).reshape(B, S, N_BR * DSW_HEADS, DSW_HEAD_DIM)
    q = rope(q, pos, DSW_ROT_DIM).reshape(B, S, N_BR, DSW_HEADS, DSW_HEAD_DIM)
    outs, lses = [], []
    for g, (window, dilation) in enumerate(DSW_BRANCHES):
        o, lse = dilated_branch(q[:, :, g], k_sh[:, :, g], v_sh[:, :, g], window, dilation)
        outs.append(o)
        lses.append(lse)
    wts = jax.nn.softmax(jnp.stack(lses, axis=0), axis=0)
    o = jnp.sum(wts[..., None].astype(xn.dtype) * jnp.stack(outs, axis=0), axis=0)
    return o.reshape(B, S, DSW_WIDTH) @ w_o


def swiglu(xn, wg, wu, wd):
    return (jax.nn.silu(xn @ wg) * (xn @ wu)) @ wd


def moe_swiglu(xn, router, wg, wu, wd):
    logits = (xn @ router).astype(jnp.float32)
    top_vals, top_idx = lax.top_k(logits, TOP_K)
    gates = jax.nn.softmax(top_vals, axis=-1)
    combine = jnp.sum(jax.nn.one_hot(top_idx, N_EXPERTS, dtype=jnp.float32) * gates[..., None], axis=-2)
    out = jnp.zeros_like(xn)
    for e in range(N_EXPERTS):
        out = out + combine[..., e:e + 1].astype(xn.dtype) * swiglu(xn, wg[e], wu[e], wd[e])
    return out


def setup_inputs(seed: int = 0) -> dict:
    key = jax.random.key(seed)
    ks = iter(jax.random.split(key, 32))

    def w(shape, fan_in):
        return jax.random.normal(next(ks), shape, jnp.float32) * (fan_in ** -0.5)

    def gain(shape):
        return 1.0 + 0.05 * jax.random.normal(next(ks), shape, jnp.float32)

    x = jax.random.normal(next(ks), (BATCH, SEQ, D_MODEL), jnp.float32)
    offs = jax.random.randint(next(ks), (BATCH, 1), 0, 8192, dtype=jnp.int32)
    positions = offs + jnp.arange(SEQ, dtype=jnp.int32)[None, :]
    return {
        "x": x,
        "positions": positions,
        "norm_attn": gain((DEPTH, D_MODEL)),
        "norm_ffn": gain((DEPTH, D_MODEL)),
        "mla_w_down": w((N_A_LAYERS, D_MODEL, MLA_DOWN_DIM), D_MODEL),
        "mla_q_norm": gain((N_A_LAYERS, MLA_Q_LORA)),
        "mla_w_uq": w((N_A_LAYERS, MLA_Q_LORA, MLA_HEADS * MLA_QK_DIM), MLA_Q_LORA),
        "mla_kv_norm": gain((N_A_LAYERS, MLA_KV_LORA)),
        "mla_w_ukv": w((N_A_LAYERS, MLA_KV_LORA, MLA_HEADS * (MLA_NOPE_DIM + MLA_V_DIM)), MLA_KV_LORA),
        "mla_w_o": w((N_A_LAYERS, MLA_HEADS * MLA_V_DIM, D_MODEL), MLA_HEADS * MLA_V_DIM),
        "dsw_kv_norm": gain((D_MODEL,)),
        "dsw_w_kv": w((D_MODEL, 2 * N_BR * DSW_WIDTH), D_MODEL),
        "dsw_w_q": w((N_B_LAYERS, D_MODEL, N_BR * DSW_WIDTH), D_MODEL),
        "dsw_w_o": w((N_B_LAYERS, DSW_WIDTH, D_MODEL), DSW_WIDTH),
        "ffn_w_gate": w((N_DENSE_FFN, D_MODEL, FFN_DIM), D_MODEL),
        "ffn_w_up": w((N_DENSE_FFN, D_MODEL, FFN_DIM), D_MODEL),
        "ffn_w_down": w((N_DENSE_FFN, FFN_DIM, D_MODEL), FFN_DIM),
        "moe_router": w((N_MOE_FFN, D_MODEL, N_EXPERTS), D_MODEL),
        "moe_w_gate": w((N_MOE_FFN, N_EXPERTS, D_MODEL, EXPERT_DIM), D_MODEL),
        "moe_w_up": w((N_MOE_FFN, N_EXPERTS, D_MODEL, EXPERT_DIM), D_MODEL),
        "moe_w_down": w((N_MOE_FFN, N_EXPERTS, EXPERT_DIM, D_MODEL), EXPERT_DIM),
        "final_norm": gain((D_MODEL,)),
    }


def reference(x, positions, norm_attn, norm_ffn, mla_w_down, mla_q_norm, mla_w_uq, mla_kv_norm,
              mla_w_ukv, mla_w_o, dsw_kv_norm, dsw_w_kv, dsw_w_q, dsw_w_o, ffn_w_gate, ffn_w_up,
              ffn_w_down, moe_router, moe_w_gate, moe_w_up, moe_w_down, final_norm):
    h = x
    k_sh = v_sh = None
    for layer in range(DEPTH):
        xn = rmsnorm(h, norm_attn[layer])
        if layer < N_A_LAYERS:
            i = layer
            h = h + mla_attention(xn, positions, mla_w_down[i], mla_q_norm[i], mla_w_uq[i],
                                  mla_kv_norm[i], mla_w_ukv[i], mla_w_o[i])
        else:
            if layer == N_A_LAYERS:
                k_sh, v_sh = shared_dsw_kv(h, positions, dsw_kv_norm, dsw_w_kv)
                xn = rmsnorm(h, norm_attn[layer])
            i = layer - N_A_LAYERS
            h = h + dsw_attention(xn, positions, k_sh, v_sh, dsw_w_q[i], dsw_w_o[i])
        hn = rmsnorm(h, norm_ffn[layer])
        j = layer // 2
        if layer % 2 == 0:
            h = h + swiglu(hn, ffn_w_gate[j], ffn_w_up[j], ffn_w_down[j])
        else:
            h = h + moe_swiglu(hn, moe_router[j], moe_w_gate[j], moe_w_up[j], moe_w_down[j])
    return rmsnorm(h, final_norm)
```

```python
import math
import os
from contextlib import ExitStack

import numpy as np
import concourse.bass as bass
import concourse.mybir as mybir
from concourse.bass_utils import run_bass_kernel_spmd

F32 = mybir.dt.float32
BF16 = mybir.dt.bfloat16
I32 = mybir.dt.int32
ALU = mybir.AluOpType
AF = mybir.ActivationFunctionType
AX = mybir.AxisListType

T = 2048
NT = 16
D = 1024
KC = 8
EPS = 1e-6
NEG = -30000.0
CAP = int(os.environ.get("MK_CAP", "640"))
NJ = CAP // 128
ENGS = ["pe", "act", "dve", "pool", "sp"]


class Op:
    __slots__ = ("eng", "fn", "reads", "writes", "dma_key", "deps", "signal", "sem", "val", "idx")


class Sched:
    def __init__(self, nc, es, tag=""):
        self.nc = nc
        self.es = es
        self.tag = tag
        self.sem = {e: es.enter_context(nc.semaphore("s_" + tag + e)) for e in ENGS}
        self.sigcount = {e: 0 for e in ENGS}
        self.waited = {e: {} for e in ENGS}
        self.dma_sem = {}
        self.dma_cnt = {}
        self.ops = []
        self.last_writer = {}
        self.readers = {}
        self.n_total = 0

    def op(self, eng, fn, reads=(), writes=(), dma_key=None):
        o = Op()
        o.eng, o.fn, o.reads, o.writes, o.dma_key = eng, fn, tuple(reads), tuple(writes), dma_key
        o.deps = set()
        o.signal = dma_key is not None
        o.sem = None
        o.val = None
        o.idx = len(self.ops)
        for k in o.reads:
            w = self.last_writer.get(k)
            if w is not None:
                o.deps.add(w)
            if isinstance(k, tuple) and k[0] in ("psf", "psb"):
                for r in self.readers.get(k, ()):
                    if r.eng != eng:
                        o.deps.add(r)
        for k in o.writes:
            w = self.last_writer.get(k)
            if w is not None:
                if dma_key is not None and w.dma_key == dma_key and w.eng == eng:
                    o.deps |= w.deps
                else:
                    o.deps.add(w)
            for r in self.readers.get(k, ()):
                o.deps.add(r)
        o.deps.discard(o)
        for k in o.reads:
            self.readers.setdefault(k, []).append(o)
        for k in o.writes:
            self.last_writer[k] = o
            self.readers[k] = []
        if dma_key is not None and dma_key not in self.dma_sem:
            self.dma_sem[dma_key] = self.es.enter_context(self.nc.semaphore("d_" + self.tag + str(len(self.dma_sem))))
            self.dma_cnt[dma_key] = 0
        self.ops.append(o)
        return o

    def barrier_waits(self, ename, eng):
        wd = self.waited[ename]
        pre_sig = getattr(self, "_pre_sig", None) or {e: 0 for e in ENGS}
        pre_dma = getattr(self, "_pre_dma", None) or {}
        for x in ENGS:
            if x != ename and pre_sig[x] > 0 and wd.get(self.sem[x].name, 0) < pre_sig[x]:
                eng.wait_ge(self.sem[x], pre_sig[x])
                wd[self.sem[x].name] = pre_sig[x]
        for k, v in pre_dma.items():
            if v > 0 and wd.get(self.dma_sem[k].name, 0) < v:
                eng.wait_ge(self.dma_sem[k], v)
                wd[self.dma_sem[k].name] = v

    def flush(self, guard=None, outer=None):
        ops = self.ops
        if not ops:
            return
        last = {}
        for o in ops:
            last[o.eng] = o
            for d in o.deps:
                if d.dma_key is None and not (d.eng == "pe" and o.eng == "pe" and o.dma_key is None):
                    d.signal = True
        for e, o in last.items():
            o.signal = True
        for o in ops:
            if o.dma_key is not None:
                self.dma_cnt[o.dma_key] += 16
                o.sem = self.dma_sem[o.dma_key]
                o.val = self.dma_cnt[o.dma_key]
            elif o.signal:
                self.sigcount[o.eng] += 1
                o.sem = self.sem[o.eng]
                o.val = self.sigcount[o.eng]
        pre_sig = dict(self._pre_sig) if hasattr(self, "_pre_sig") else {e: 0 for e in ENGS}
        pre_dma = dict(self._pre_dma) if hasattr(self, "_pre_dma") else {}
        by_eng = {e: [o for o in ops if o.eng == e] for e in ENGS}

        def emit(ename, eng):
            wd = self.waited[ename]

            def wait(sem, val):
                if val <= 0:
                    return
                if wd.get(sem.name, 0) < val:
                    eng.wait_ge(sem, val)
                    wd[sem.name] = val

            for x in ENGS:
                if x != ename:
                    wait(self.sem[x], pre_sig[x])
            for k, v in pre_dma.items():
                wait(self.dma_sem[k], v)
            for o in by_eng[ename]:
                for d in sorted(o.deps, key=lambda z: z.idx):
                    if d.dma_key is None and d.eng == "pe" and ename == "pe" and o.dma_key is None:
                        continue
                    wait(d.sem, d.val)
                ins = o.fn(eng)
                if o.signal:
                    ins.then_inc(o.sem, 16 if o.dma_key is not None else 1)

        def emit_guarded(ename, eng):
            outer.barrier_waits(ename, eng)
            reg = eng.alloc_register("flag_%s_%d" % (ename, id(self) % 100000))
            eng.reg_load(reg, guard)
            with eng.If(eng.snap(reg) > 0):
                emit(ename, eng)
                for x in ENGS:
                    if x != ename and self.sigcount[x] > 0:
                        eng.wait_ge(self.sem[x], self.sigcount[x])
                for k, v in self.dma_cnt.items():
                    if v > 0:
                        eng.wait_ge(self.dma_sem[k], v)

        with self.nc.Block() as block:
            if guard is not None:
                block.tensor(lambda eng: emit_guarded("pe", eng))
                block.scalar(lambda eng: emit_guarded("act", eng))
                block.vector(lambda eng: emit_guarded("dve", eng))
                block.gpsimd(lambda eng: emit_guarded("pool", eng))
                block.sync(lambda eng: emit_guarded("sp", eng))
            else:
                if by_eng["pe"]:
                    block.tensor(lambda eng: emit("pe", eng))
                if by_eng["act"]:
                    block.scalar(lambda eng: emit("act", eng))
                if by_eng["dve"]:
                    block.vector(lambda eng: emit("dve", eng))
                if by_eng["pool"]:
                    block.gpsimd(lambda eng: emit("pool", eng))
                if by_eng["sp"]:
                    block.sync(lambda eng: emit("sp", eng))
        self._pre_sig = dict(self.sigcount)
        self._pre_dma = dict(self.dma_cnt)
        self.n_total += len(ops)
        self.ops = []
        self.last_writer = {}
        self.readers = {}

    def final_wait(self):
        pre_sig = dict(self._pre_sig)
        pre_dma = dict(self._pre_dma)

        def emit(ename, eng):
            for x in ENGS:
                if x != ename and pre_sig[x] > 0:
                    eng.wait_ge(self.sem[x], pre_sig[x])
            for k, v in pre_dma.items():
                if v > 0:
                    eng.wait_ge(self.dma_sem[k], v)

        with self.nc.Block() as block:
            block.sync(lambda eng: emit("sp", eng))
            block.vector(lambda eng: emit("dve", eng))


def host_consts():
    p = np.arange(128)[:, None]
    j = np.arange(128)[None, :]
    dif = j - p

    def m(cond):
        a = np.where(cond, 0.0, NEG).astype(np.float32)
        return np.tile(a, (1, 4))

    masks = np.stack(
        [
            m(dif >= 0),
            m(dif <= 0),
            m((dif >= 0) & (dif % 4 == 0)),
            m(dif % 4 == 0),
            m((dif <= 0) & (dif % 4 == 0)),
            m((dif >= 0) & (dif % 16 == 0)),
            m(dif % 16 == 0),
        ],
        axis=1,
    )
    masks01 = (masks == 0.0).astype(np.float32)
    ident = np.eye(128, dtype=np.float32)
    theta = np.float32(500000.0)
    f_mla = np.power(theta, -np.arange(16, dtype=np.float32) * np.float32(2.0 / 32)).astype(np.float32)
    f_dsw = np.power(theta, -np.arange(8, dtype=np.float32) * np.float32(2.0 / 16)).astype(np.float32)
    freqs = np.concatenate([f_mla, f_dsw])[None, :].repeat(128, 0).astype(np.float32)
    ltri = (np.arange(128)[:, None] < np.arange(128)[None, :]).astype(np.float32)
    iota = np.arange(CAP, dtype=np.float32)[None, :].repeat(128, 0)
    return {"c_masks": np.ascontiguousarray(masks01), "c_ident": ident, "c_freqs": np.ascontiguousarray(freqs),
            "c_ltri": np.ascontiguousarray(ltri), "c_iota": np.ascontiguousarray(iota)}


STOP = os.environ.get("MK_STOP", "")


def build_program(stop=""):
    nc = bass.Bass("TRN2", target_bir_lowering=False)

    def din(name, shape, dt=F32):
        return nc.dram_tensor(name, list(shape), dt, kind="ExternalInput").ap()

    x_d = din("x", [T, D])
    pos_d = din("pos", [128, NT], I32)
    norm_attn = din("norm_attn", [2, D])
    norm_ffn = din("norm_ffn", [2, D])
    w_down = din("mla_w_down", [D, 672])
    q_norm = din("mla_q_norm", [1, 384])
    w_uq = din("mla_w_uq", [384, 1536])
    kv_norm = din("mla_kv_norm", [1, 256])
    w_ukv = din("mla_w_ukv", [256, 2048])
    w_o_a = din("mla_w_o", [D, D])
    dsw_kv_norm = din("dsw_kv_norm", [1, D])
    w_kv = din("dsw_w_kv", [D, 6144])
    w_q = din("dsw_w_q", [D, 3072])
    w_o_b = din("dsw_w_o", [D, D])
    ffn_wg = din("ffn_w_gate", [D, 2816])
    ffn_wu = din("ffn_w_up", [D, 2816])
    ffn_wd = din("ffn_w_down", [2816, D])
    router = din("moe_router", [D, 8])
    moe_wg = din("moe_w_gate", [8, D, 3584])
    moe_wu = din("moe_w_up", [8, D, 3584])
    moe_wd = din("moe_w_down", [8, 3584, D])
    final_norm = din("final_norm", [1, D])
    c_masks = din("c_masks", [128, 7, 512])
    c_ident = din("c_ident", [128, 128])
    c_freqs = din("c_freqs", [128, 24])
    c_ltri = din("c_ltri", [128, 128])
    c_iota = din("c_iota", [128, CAP])
    flag_d = nc.dram_tensor("moe_flag", [1, 8], I32, kind="Internal").ap()
    out_d = nc.dram_tensor("out", [T, D], F32, kind="ExternalOutput").ap()

    es = ExitStack()
    S_main = Sched(nc, es)
    cur = [S_main]

    class _Proxy:
        def op(self, *a, **k):
            return cur[0].op(*a, **k)

        def flush(self, *a, **k):
            return cur[0].flush(*a, **k)

        def final_wait(self):
            return cur[0].final_wait()

    S = _Proxy()

    uid = [0]

    def sb(name, shape, dt, stack=None):
        uid[0] += 1
        return (stack or es).enter_context(nc.sbuf_tensor("%s_%d" % (name, uid[0]), list(shape), dt))

    def psum(name, shape, dt, stack=None):
        return (stack or es).enter_context(nc.psum_tensor(name, list(shape), dt))

    h = sb("h", [128, NT, D], F32)
    ident_b = sb("ident_b", [128, 128], BF16)
    ident_f = sb("ident_f", [128, 128], F32)
    freqs = sb("freqs", [128, 24], F32)
    posf = sb("posf", [128, NT], F32)
    posi = sb("posi", [128, NT], I32)
    ss = sb("ss", [128, NT], F32)
    rstd = sb("rstd", [128, NT], F32)
    sqj = sb("sqj", [128, D], BF16)
    pibias = sb("pibias", [128, 1], F32)
    epsb = sb("epsb", [128, 1], F32)
    es_attn = ExitStack()
    masks = sb("masks", [128, 7, 512], BF16, es_attn)
    cs_mla = sb("cs_mla", [128, NT, 2, 16], F32, es_attn)
    cs_dsw = sb("cs_dsw", [128, NT, 2, 8], F32, es_attn)
    xnb = [sb("xnb%d" % i, [128, D], BF16, es_attn) for i in range(2)]
    gbc_ref = [sb("gbc", [128, D], F32, es_attn)]
    es_init = ExitStack()
    rq_i = sb("rq_i", [128, 256], I32, es_init)
    rq_f = sb("rq_f", [128, 256], F32, es_init)
    rq_m = sb("rq_m", [128, 256], F32, es_init)

    psf = [psum("psf%d" % i, [128, 512], F32) for i in range(6)]
    psb = [psum("psb%d" % i, [128, 1024], BF16) for i in range(2)]

    cnt = {"psf": 0, "psb": 0, "xnb": 0, "pso": 0}

    def next_psf():
        i = cnt["psf"] % 4
        cnt["psf"] += 1
        return i

    def next_pso():
        i = 4 + cnt["pso"] % 2
        cnt["pso"] += 1
        return i

    def next_psb():
        i = cnt["psb"] % 2
        cnt["psb"] += 1
        return i

    class Pipe:
        def __init__(self, L):
            self.L = L
            self.q = []

        def push(self, front, back):
            front()
            self.q.append(back)
            while len(self.q) > self.L:
                self.q.pop(0)()

        def drain(self):
            while self.q:
                self.q.pop(0)()

    def dma_sp(out, in_, key, reads=(), writes=(), slow=False):
        if slow:
            S.op("sp", lambda e: e.dma_start(out=out, in_=in_, allow_slow_non_contiguous=True), reads=reads, writes=writes, dma_key=key)
        else:
            S.op("sp", lambda e: e.dma_start(out=out, in_=in_), reads=reads, writes=writes, dma_key=key)

    def dma_cast(out, in_, key, reads=(), writes=()):
        S.op("pool", lambda e: e.dma_start(out=out, in_=in_), reads=reads, writes=writes, dma_key=key)

    def load_w(dst, src, key, nchunk, rows=128):
        v = src.rearrange("(c p) n -> p c n", p=rows)
        for c in range(nchunk):
            dma_cast(dst[:, c, :], v[:, c, :], key, writes=[key])

    def load_gain(src_row):
        dma_sp(gbc_ref[0][:], src_row.partition_broadcast(128), "gbc", writes=["gbc"])

    def norm_stats():
        for t in range(NT):
            S.op(
                "act",
                lambda e, t=t: e.activation(out=sqj[:], in_=h[:, t, :], func=AF.Square, accum_out=ss[:, t : t + 1]),
                reads=[("h", t)],
                writes=["sqj", ("ss", t)],
            )
        allss = [("ss", t) for t in range(NT)]
        allr = [("rstd", t) for t in range(NT)]
        S.op("act", lambda e: e.activation(out=rstd[:], in_=ss[:], func=AF.Sqrt, bias=epsb[:], scale=1.0 / D), reads=allss + ["epsb"], writes=allr)
        S.op("dve", lambda e: e.reciprocal(out=rstd[:], in_=rstd[:]), reads=allr, writes=allr)

    def norm_to_T(dstT, dkey, use_gain=True):
        for t in range(NT):
            b = cnt["xnb"] % 2
            cnt["xnb"] += 1
            if use_gain:
                S.op(
                    "dve",
                    lambda e, t=t, b=b: e.scalar_tensor_tensor(
                        out=xnb[b][:], in0=h[:, t, :], scalar=rstd[:, t : t + 1], in1=gbc_ref[0][:], op0=ALU.mult, op1=ALU.mult
                    ),
                    reads=[("h", t), ("rstd", t), "gbc"],
                    writes=[("xnb", b)],
                )
            else:
                S.op(
                    "dve",
                    lambda e, t=t, b=b: e.tensor_scalar(
                        out=xnb[b][:], in0=h[:, t, :], scalar1=rstd[:, t : t + 1], scalar2=None, op0=ALU.mult
                    ),
                    reads=[("h", t), ("rstd", t)],
                    writes=[("xnb", b)],
                )
            pb = next_psb()
            for k in range(KC):
                S.op(
                    "pe",
                    lambda e, k=k, b=b, pb=pb: e.transpose(psb[pb][:, k * 128 : (k + 1) * 128], xnb[b][:, k * 128 : (k + 1) * 128], ident_b[:]),
                    reads=[("xnb", b), "ident_b"],
                    writes=[("psb", pb)],
                )
            S.op(
                "act",
                lambda e, t=t, pb=pb: e.copy(out=dstT[:, :, t * 128 : (t + 1) * 128], in_=psb[pb][:].rearrange("p (k c) -> p k c", k=KC)),
                reads=[("psb", pb)],
                writes=[(dkey, t)],
            )

    def add_proj_to_h(srcT, skey, wt, wkey, nk, scale_col=None):
        for t in range(NT):
            for half in range(2):
                pi = next_psf()
                for k in range(nk):
                    S.op(
                        "pe",
                        lambda e, k=k, t=t, half=half, pi=pi: e.matmul(
                            psf[pi][:],
                            srcT[:, k, t * 128 : (t + 1) * 128],
                            wt[:, k, half * 512 : (half + 1) * 512],
                            start=(k == 0),
                            stop=(k == nk - 1),
                        ),
                        reads=[(skey, t), wkey],
                        writes=[("psf", pi)],
                    )
                hs = h[:, t, half * 512 : (half + 1) * 512]
                if scale_col is None:
                    S.op(
                        "dve",
                        lambda e, hs=hs, pi=pi: e.tensor_tensor(out=hs, in0=psf[pi][:], in1=hs, op=ALU.add),
                        reads=[("psf", pi), ("h", t)],
                        writes=[("h", t)],
                    )
                else:
                    sc, sckey = scale_col(t)
                    S.op(
                        "dve",
                        lambda e, hs=hs, pi=pi, sc=sc: e.scalar_tensor_tensor(
                            out=hs, in0=psf[pi][:], scalar=sc, in1=hs, op0=ALU.mult, op1=ALU.add
                        ),
                        reads=[("psf", pi), ("h", t), sckey],
                        writes=[("h", t)],
                    )

    def swiglu_bufs(st, GS=4):
        return dict(
            GS=GS,
            wg=[sb("wg%d" % i, [128, KC, GS * 128], BF16, st) for i in range(2)],
            wu=[sb("wu%d" % i, [128, KC, GS * 128], BF16, st) for i in range(2)],
            wd=[sb("wd%d" % i, [128, GS, D], BF16, st) for i in range(2)],
            hT=sb("hT", [128, GS, T], BF16, st),
            sil=[sb("sil%d" % i, [128, 512], F32, st) for i in range(2)],
            cnt=[0],
        )

    def swiglu_block(xT, xkey, wg_src, wu_src, wd_src, F, st, scale_col=None, bufs=None):
        if bufs is None:
            bufs = swiglu_bufs(st)
        GS = bufs["GS"]
        nF = F // 128
        groups = [(g0, min(GS, nF - g0)) for g0 in range(0, nF, GS)]
        wg, wu, wd, hT, sil = bufs["wg"], bufs["wu"], bufs["wd"], bufs["hT"], bufs["sil"]
        base = bufs["cnt"][0]
        bufs["cnt"][0] += len(groups)
        wgv = wg_src.rearrange("(c p) n -> p c n", p=128)
        wuv = wu_src.rearrange("(c p) n -> p c n", p=128)
        wdv = wd_src.rearrange("(c p) n -> p c n", p=128)

        def load_group(gi):
            g0, G = groups[gi]
            s = (base + gi) % 2
            for c in range(KC):
                dma_cast(wg[s][:, c, : G * 128], wgv[:, c, g0 * 128 : (g0 + G) * 128], ("wg", s), writes=[("wg", s)])
            for c in range(KC):
                dma_cast(wu[s][:, c, : G * 128], wuv[:, c, g0 * 128 : (g0 + G) * 128], ("wu", s), writes=[("wu", s)])
            for c in range(G):
                dma_cast(wd[s][:, c, :], wdv[:, g0 + c, :], ("wd", s), writes=[("wd", s)])

        load_group(0)
        nsil = 0
        for gi, (g0, G) in enumerate(groups):
            s = (base + gi) % 2
            if gi + 1 < len(groups):
                load_group(gi + 1)
            for fc in range(G):
                for nb in range(4):
                    pg = next_psf()
                    pu = next_psf()
                    for k in range(KC):
                        S.op(
                            "pe",
                            lambda e, k=k, fc=fc, nb=nb, pg=pg, s=s: e.matmul(
                                psf[pg][:], wg[s][:, k, fc * 128 : (fc + 1) * 128], xT[:, k, nb * 512 : (nb + 1) * 512],
                                start=(k == 0), stop=(k == KC - 1),
                            ),
                            reads=[("wg", s)] + [(xkey, nb * 4 + i) for i in range(4)],
                            writes=[("psf", pg)],
                        )
                    for k in range(KC):
                        S.op(
                            "pe",
                            lambda e, k=k, fc=fc, nb=nb, pu=pu, s=s: e.matmul(
                                psf[pu][:], wu[s][:, k, fc * 128 : (fc + 1) * 128], xT[:, k, nb * 512 : (nb + 1) * 512],
                                start=(k == 0), stop=(k == KC - 1),
                            ),
                            reads=[("wu", s)] + [(xkey, nb * 4 + i) for i in range(4)],
                            writes=[("psf", pu)],
                        )
                    si = nsil % 2
                    nsil += 1
                    S.op(
                        "act",
                        lambda e, pg=pg, si=si: e.activation(out=sil[si][:], in_=psf[pg][:], func=AF.Silu),
                        reads=[("psf", pg)],
                        writes=[("sil", si)],
                    )
                    S.op(
                        "dve",
                        lambda e, pu=pu, si=si, fc=fc, nb=nb: e.tensor_tensor(
                            out=hT[:, fc, nb * 512 : (nb + 1) * 512], in0=psf[pu][:], in1=sil[si][:], op=ALU.mult
                        ),
                        reads=[("psf", pu), ("sil", si)],
                        writes=[("hT", nb * 4 + i) for i in range(4)],
                    )
            add_proj_to_h(hT, "hT", wd[s], ("wd", s), G, scale_col=scale_col)

    for t in range(NT):
        dma_sp(h[:, t, :], x_d[t * 128 : (t + 1) * 128, :], ("h_in", t), writes=[("h", t)])
    dma_cast(ident_b[:], c_ident, "ident_b", writes=["ident_b"])
    dma_sp(ident_f[:], c_ident, "ident_f", writes=["ident_f"])
    dma_cast(masks[:], c_masks, "masks", writes=["masks"])
    dma_sp(freqs[:], c_freqs, "freqs", writes=["freqs"])
    dma_sp(posi[:], pos_d, "posi", writes=["posi"])
    S.op("dve", lambda e: e.tensor_copy(out=posf[:], in_=posi[:]), reads=["posi"], writes=["posf"])
    S.op("dve", lambda e: e.memset(pibias[:], math.pi), writes=["pibias"])
    S.op("dve", lambda e: e.memset(epsb[:], EPS), writes=["epsb"])


    def rope_table(cs, nf, f0, key, ntab=NT, pcol0=0):
        n = ntab * nf
        pb_ = posf[:, pcol0:pcol0 + ntab].unsqueeze(2).to_broadcast([128, ntab, nf])
        fb_ = freqs[:, f0:f0 + nf].unsqueeze(1).to_broadcast([128, ntab, nf])
        rf = rq_f[:, 0:n].rearrange("p (t f) -> p t f", f=nf)
        ri = rq_i[:, 0:n].rearrange("p (t f) -> p t f", f=nf)
        rm = rq_m[:, 0:n].rearrange("p (t f) -> p t f", f=nf)
        for which in range(2):
            dst = cs[:, 0:ntab, which, :]
            wk = [(key, "w", which)]
            S.op("dve", lambda e, dst=dst: e.tensor_tensor(out=dst, in0=pb_, in1=fb_, op=ALU.mult), reads=["freqs", "posf"], writes=wk)
            if which == 0:
                S.op("dve", lambda e, dst=dst: e.tensor_scalar(out=dst, in0=dst, scalar1=math.pi / 2, scalar2=None, op0=ALU.add), reads=wk, writes=wk)
            S.op("dve", lambda e, dst=dst: e.tensor_scalar(out=rf, in0=dst, scalar1=1.0 / (2 * math.pi), scalar2=None, op0=ALU.mult), reads=wk, writes=["rq_f"])
            S.op("dve", lambda e: e.tensor_copy(out=ri, in_=rf), reads=["rq_f"], writes=["rq_i"])
            S.op("dve", lambda e: e.tensor_copy(out=rf, in_=ri), reads=["rq_i"], writes=["rq_f"])
            S.op("dve", lambda e, dst=dst: e.scalar_tensor_tensor(out=dst, in0=rf, scalar=-2 * math.pi, in1=dst, op0=ALU.mult, op1=ALU.add), reads=["rq_f"] + wk, writes=wk)
            S.op("dve", lambda e, dst=dst: e.tensor_scalar(out=rm, in0=dst, scalar1=math.pi, scalar2=-2 * math.pi, op0=ALU.is_gt, op1=ALU.mult), reads=wk, writes=["rq_m"])
            S.op("dve", lambda e, dst=dst: e.tensor_tensor(out=dst, in0=dst, in1=rm, op=ALU.add), reads=["rq_m"] + wk, writes=wk)
            S.op("dve", lambda e, dst=dst: e.tensor_scalar(out=rm, in0=dst, scalar1=-math.pi, scalar2=2 * math.pi, op0=ALU.is_lt, op1=ALU.mult), reads=wk, writes=["rq_m"])
            S.op("dve", lambda e, dst=dst: e.tensor_tensor(out=dst, in0=dst, in1=rm, op=ALU.add), reads=["rq_m"] + wk, writes=wk)
            S.op("act", lambda e, dst=dst: e.activation(out=dst, in_=dst, func=AF.Sin), reads=wk, writes=wk + [(key, t, which) for t in range(ntab)])

    rope_table(cs_mla, 16, 0, "cs_mla")
    rope_table(cs_dsw, 8, 16, "cs_dsw")
    S.flush()
    es_init.close()

    def phase_mla():
        st = ExitStack()
        cqT = sb("cqT", [128, 3, T], BF16, st)
        ckvT = sb("ckvT", [128, 2, T], BF16, st)
        krb = sb("krb", [128, NT, 32], BF16, st)
        with ExitStack() as st1:
            xnT = sb("xnT", [128, KC, T], BF16, st1)
            wdn = sb("wdn", [128, KC, 672], BF16, st1)
            gq = sb("gq", [128, 384], F32, st1)
            gkv = sb("gkv", [128, 256], F32, st1)
            dn = [sb("dn%d" % i, [128, 672], F32, st1) for i in range(2)]
            cb = [sb("cb%d" % i, [128, 640], BF16, st1) for i in range(2)]
            s2 = sb("s2", [128, NT, 2], F32, st1)
            r2 = sb("r2", [128, NT, 2], F32, st1)
            rt = [sb("rt%d" % i, [128, 4, 16], F32, st1) for i in range(2)]
            load_w(wdn, w_down, "wdn", KC)
            load_gain(norm_attn[0:1, :])
            dma_sp(gq[:], q_norm.partition_broadcast(128), "gq", writes=["gq"])
            dma_sp(gkv[:], kv_norm.partition_broadcast(128), "gkv", writes=["gkv"])
            norm_stats()
            norm_to_T(xnT, "xnT")
            for t in range(NT):
                b = t % 2
                p0 = next_psf()
                p1 = next_psf()
                for k in range(KC):
                    S.op("pe", lambda e, k=k, t=t, p0=p0: e.matmul(psf[p0][:], xnT[:, k, t * 128:(t + 1) * 128], wdn[:, k, 0:512], start=(k == 0), stop=(k == KC - 1)),
                         reads=[("xnT", t), "wdn"], writes=[("psf", p0)])
                for k in range(KC):
                    S.op("pe", lambda e, k=k, t=t, p1=p1: e.matmul(psf[p1][:, 0:160], xnT[:, k, t * 128:(t + 1) * 128], wdn[:, k, 512:672], start=(k == 0), stop=(k == KC - 1)),
                         reads=[("xnT", t), "wdn"], writes=[("psf", p1)])
                S.op("act", lambda e, p0=p0, b=b: e.copy(out=dn[b][:, 0:512], in_=psf[p0][:]), reads=[("psf", p0)], writes=[("dn", b, 0)])
                S.op("act", lambda e, p1=p1, b=b: e.copy(out=dn[b][:, 512:672], in_=psf[p1][:, 0:160]), reads=[("psf", p1)], writes=[("dn", b, 1)])
                S.op("act", lambda e, b=b, t=t: e.activation(out=sqj[:, 0:384], in_=dn[b][:, 0:384], func=AF.Square, accum_out=s2[:, t, 0:1]),
                     reads=[("dn", b, 0)], writes=["sqj", ("s2", t, 0)])
                S.op("act", lambda e, b=b, t=t: e.activation(out=sqj[:, 384:640], in_=dn[b][:, 384:640], func=AF.Square, accum_out=s2[:, t, 1:2]),
                     reads=[("dn", b, 0), ("dn", b, 1)], writes=["sqj", ("s2", t, 1)])
                S.op("dve", lambda e, t=t: e.tensor_scalar(out=r2[:, t, 0:1], in0=s2[:, t, 0:1], scalar1=1.0 / 384, scalar2=EPS, op0=ALU.mult, op1=ALU.add),
                     reads=[("s2", t, 0)], writes=[("r2", t, 0)])
                S.op("dve", lambda e, t=t: e.tensor_scalar(out=r2[:, t, 1:2], in0=s2[:, t, 1:2], scalar1=1.0 / 256, scalar2=EPS, op0=ALU.mult, op1=ALU.add),
                     reads=[("s2", t, 1)], writes=[("r2", t, 1)])
                S.op("act", lambda e, t=t: e.activation(out=r2[:, t, :], in_=r2[:, t, :], func=AF.Sqrt),
                     reads=[("r2", t, 0), ("r2", t, 1)], writes=[("r2", t, 0), ("r2", t, 1)])
                S.op("dve", lambda e, t=t: e.reciprocal(out=r2[:, t, :], in_=r2[:, t, :]),
                     reads=[("r2", t, 0), ("r2", t, 1)], writes=[("r2", t, 0), ("r2", t, 1)])
                S.op("dve", lambda e, b=b, t=t: e.scalar_tensor_tensor(out=cb[b][:, 0:384], in0=dn[b][:, 0:384], scalar=r2[:, t, 0:1], in1=gq[:], op0=ALU.mult, op1=ALU.mult),
                     reads=[("dn", b, 0), ("r2", t, 0), "gq"], writes=[("cb", b)])
                S.op("dve", lambda e, b=b, t=t: e.scalar_tensor_tensor(out=cb[b][:, 384:640], in0=dn[b][:, 384:640], scalar=r2[:, t, 1:2], in1=gkv[:], op0=ALU.mult, op1=ALU.mult),
                     reads=[("dn", b, 0), ("dn", b, 1), ("r2", t, 1), "gkv"], writes=[("cb", b)])
                x1 = dn[b][:, 640:656]
                x2 = dn[b][:, 656:672]
                co = cs_mla[:, t, 0, :]
                si = cs_mla[:, t, 1, :]
                S.op("dve", lambda e, b=b, x1=x1, co=co: e.tensor_tensor(out=rt[b][:, 0, :], in0=x1, in1=co, op=ALU.mult), reads=[("dn", b, 1), ("cs_mla", t, 0)], writes=[("rt", b)])
                S.op("dve", lambda e, b=b, x2=x2, si=si: e.tensor_tensor(out=rt[b][:, 1, :], in0=x2, in1=si, op=ALU.mult), reads=[("dn", b, 1), ("cs_mla", t, 1)], writes=[("rt", b)])
                S.op("dve", lambda e, b=b, x2=x2, co=co: e.tensor_tensor(out=rt[b][:, 2, :], in0=x2, in1=co, op=ALU.mult), reads=[("dn", b, 1), ("cs_mla", t, 0)], writes=[("rt", b)])
                S.op("dve", lambda e, b=b, x1=x1, si=si: e.tensor_tensor(out=rt[b][:, 3, :], in0=x1, in1=si, op=ALU.mult), reads=[("dn", b, 1), ("cs_mla", t, 1)], writes=[("rt", b)])
                S.op("dve", lambda e, b=b, t=t: e.tensor_tensor(out=krb[:, t, 0:16], in0=rt[b][:, 0, :], in1=rt[b][:, 1, :], op=ALU.subtract), reads=[("rt", b)], writes=[("krb", t)])
                S.op("dve", lambda e, b=b, t=t: e.tensor_tensor(out=krb[:, t, 16:32], in0=rt[b][:, 2, :], in1=rt[b][:, 3, :], op=ALU.add), reads=[("rt", b)], writes=[("krb", t)])
                pb = next_psb()
                for k in range(5):
                    S.op("pe", lambda e, k=k, b=b, pb=pb: e.transpose(psb[pb][:, k * 128:(k + 1) * 128], cb[b][:, k * 128:(k + 1) * 128], ident_b[:]),
                         reads=[("cb", b), "ident_b"], writes=[("psb", pb)])
                S.op("act", lambda e, t=t, pb=pb: e.copy(out=cqT[:, :, t * 128:(t + 1) * 128], in_=psb[pb][:, 0:384].rearrange("p (k c) -> p k c", k=3)),
                     reads=[("psb", pb)], writes=[("cqT", t)])
                S.op("act", lambda e, t=t, pb=pb: e.copy(out=ckvT[:, :, t * 128:(t + 1) * 128], in_=psb[pb][:, 384:640].rearrange("p (k c) -> p k c", k=2)),
                     reads=[("psb", pb)], writes=[("ckvT", t)])
            S.flush()
        if stop == "mla_a":
            st.close()
            return
        with ExitStack() as st2:
            wuq = sb("wuq", [128, 3, 1536], BF16, st2)
            wukv = sb("wukv", [128, 2, 2048], BF16, st2)
            woa = sb("woa", [128, KC, D], BF16, st2)
            OT = sb("OT", [128, KC, T], BF16, st2)
            qtok = [sb("qtok%d" % i, [128, 2, 96], BF16, st2) for i in range(3)]
            ktok = [sb("ktok%d" % i, [128, 2, 96], BF16, st2) for i in range(3)]
            qrt = [sb("qrt%d" % i, [128, 4, 2, 16], F32, st2) for i in range(3)]
            QT = sb("QT", [96, 2, T], BF16, st2)
            KT = sb("KT", [96, 2, T], BF16, st2)
            VA = sb("VA", [128, NT, 2, 128], BF16, st2)
            PT = [sb("PT%d" % i, [128, 512], BF16, st2) for i in range(4)]
            rec = [sb("rec%d" % i, [64, 512], F32, st2) for i in range(2)]
            load_w(wuq, w_uq, "wuq", 3)
            load_w(wukv, w_ukv, "wukv", 2)
            load_w(woa, w_o_a, "woa", KC)
            S.op("dve", lambda e: e.memset(VA[:], 1.0), writes=["VA1"])
            npt = 0
            sc = 1.0 / math.sqrt(96.0)
            for hp in range(int(os.environ.get("MK_HP", "8"))):
                tpipe = Pipe(2)
                for t in range(NT):
                  def front_t(t=t, hp=hp):
                    b = t % 3
                    pq = next_psf()
                    for k in range(3):
                        S.op("pe", lambda e, k=k, t=t, pq=pq, hp=hp: e.matmul(psf[pq][:, 0:192], cqT[:, k, t * 128:(t + 1) * 128], wuq[:, k, hp * 192:(hp + 1) * 192], start=(k == 0), stop=(k == 2)),
                             reads=[("cqT", t), "wuq"], writes=[("psf", pq)])
                    pk = next_psf()
                    for k in range(2):
                        S.op("pe", lambda e, k=k, t=t, pk=pk, hp=hp: e.matmul(psf[pk][:, 0:256], ckvT[:, k, t * 128:(t + 1) * 128], wukv[:, k, hp * 256:(hp + 1) * 256], start=(k == 0), stop=(k == 1)),
                             reads=[("ckvT", t), "wukv"], writes=[("psf", pk)])
                    q3 = psf[pq][:, 0:192].rearrange("p (h d) -> p h d", h=2)
                    kv3 = psf[pk][:, 0:256].rearrange("p (h d) -> p h d", h=2)
                    S.op("act", lambda e, b=b, q3=q3: e.copy(out=qtok[b][:, :, 0:64], in_=q3[:, :, 0:64]), reads=[("psf", pq)], writes=[("qtok", b)])
                    co = cs_mla[:, t, 0:1, :].to_broadcast([128, 2, 16])
                    si = cs_mla[:, t, 1:2, :].to_broadcast([128, 2, 16])
                    x1 = q3[:, :, 64:80]
                    x2 = q3[:, :, 80:96]
                    S.op("dve", lambda e, b=b, x1=x1, co=co: e.tensor_tensor(out=qrt[b][:, 0], in0=x1, in1=co, op=ALU.mult), reads=[("psf", pq), ("cs_mla", t, 0)], writes=[("qrt", b)])
                    S.op("dve", lambda e, b=b, x2=x2, si=si: e.tensor_tensor(out=qrt[b][:, 1], in0=x2, in1=si, op=ALU.mult), reads=[("psf", pq), ("cs_mla", t, 1)], writes=[("qrt", b)])
                    S.op("dve", lambda e, b=b, x2=x2, co=co: e.tensor_tensor(out=qrt[b][:, 2], in0=x2, in1=co, op=ALU.mult), reads=[("psf", pq), ("cs_mla", t, 0)], writes=[("qrt", b)])
                    S.op("dve", lambda e, b=b, x1=x1, si=si: e.tensor_tensor(out=qrt[b][:, 3], in0=x1, in1=si, op=ALU.mult), reads=[("psf", pq), ("cs_mla", t, 1)], writes=[("qrt", b)])
                    S.op("dve", lambda e, b=b: e.tensor_tensor(out=qtok[b][:, :, 64:80], in0=qrt[b][:, 0], in1=qrt[b][:, 1], op=ALU.subtract), reads=[("qrt", b)], writes=[("qtok", b)])
                    S.op("dve", lambda e, b=b: e.tensor_tensor(out=qtok[b][:, :, 80:96], in0=qrt[b][:, 2], in1=qrt[b][:, 3], op=ALU.add), reads=[("qrt", b)], writes=[("qtok", b)])
                    S.op("act", lambda e, b=b, kv3=kv3: e.copy(out=ktok[b][:, :, 0:64], in_=kv3[:, :, 0:64]), reads=[("psf", pk)], writes=[("ktok", b)])
                    S.op("dve", lambda e, b=b, t=t: e.tensor_copy(out=ktok[b][:, :, 64:96], in_=krb[:, t:t + 1, :].to_broadcast([128, 2, 32])), reads=[("krb", t)], writes=[("ktok", b)])
                    S.op("dve", lambda e, t=t, kv3=kv3: e.tensor_copy(out=VA[:, t, :, 0:64], in_=kv3[:, :, 64:128]), reads=[("psf", pk), "VA1"], writes=[("VA", t)])
                  def back_t(t=t):
                    b = t % 3
                    pb = next_psb()
                    for hh in range(2):
                        S.op("pe", lambda e, hh=hh, b=b, pb=pb: e.transpose(psb[pb][0:96, hh * 128:(hh + 1) * 128], qtok[b][:, hh, :], ident_b[:]),
                             reads=[("qtok", b), "ident_b"], writes=[("psb", pb)])
                    for hh in range(2):
                        S.op("pe", lambda e, hh=hh, b=b, pb=pb: e.transpose(psb[pb][0:96, 256 + hh * 128:256 + (hh + 1) * 128], ktok[b][:, hh, :], ident_b[:]),
                             reads=[("ktok", b), "ident_b"], writes=[("psb", pb)])
                    S.op("act", lambda e, t=t, pb=pb: e.copy(out=QT[:, :, t * 128:(t + 1) * 128], in_=psb[pb][0:96, 0:256].rearrange("p (h c) -> p h c", h=2)),
                         reads=[("psb", pb)], writes=[("QT", t)])
                    S.op("act", lambda e, t=t, pb=pb: e.copy(out=KT[:, :, t * 128:(t + 1) * 128], in_=psb[pb][0:96, 256:512].rearrange("p (h c) -> p h c", h=2)),
                         reads=[("psb", pb)], writes=[("KT", t)])
                  tpipe.push(front_t, back_t)
                tpipe.drain()
                apipe = Pipe(3)
                for hh in range(2 if os.environ.get("MK_ATT", "1") == "1" else 0):
                    for qb in range(int(os.environ.get("MK_QB", "4"))):
                        po = next_pso()
                        nkt = 4 * qb + 4
                        for kt in range(nkt):
                            c0 = max(0, kt - 4 * qb) * 128
                            diag = kt >= 4 * qb
                            psi = next_psf()
                            pt = npt % 4
                            npt += 1

                            def front(hh=hh, qb=qb, kt=kt, c0=c0, psi=psi, diag=diag, pt=pt):
                                S.op("pe", lambda e: e.matmul(
                                    psf[psi][:, c0:512], KT[:, hh, kt * 128:(kt + 1) * 128], QT[:, hh, qb * 512 + c0:(qb + 1) * 512], start=True, stop=True),
                                    reads=[("KT", kt)] + [("QT", qb * 4 + i) for i in range(4)], writes=[("psf", psi)])
                                S.op("act", lambda e: e.activation(out=PT[pt][:, c0:512], in_=psf[psi][:, c0:512], func=AF.Exp, scale=sc),
                                     reads=[("psf", psi)], writes=[("PT", pt)])
                                if diag:
                                    S.op("dve", lambda e: e.tensor_tensor(out=PT[pt][:, c0:c0 + 128], in0=PT[pt][:, c0:c0 + 128], in1=masks[:, 0, 0:128], op=ALU.mult),
                                         reads=[("PT", pt), "masks"], writes=[("PT", pt)])

                            def back(hh=hh, qb=qb, kt=kt, c0=c0, po=po, pt=pt, nkt=nkt, hp=hp):
                                S.op("pe", lambda e: e.matmul(
                                    psf[po][:, c0:512], VA[:, kt, hh, :], PT[pt][:, c0:512], start=(kt == 0), stop=(kt == nkt - 1)),
                                    reads=[("VA", kt), "VA1", ("PT", pt)], writes=[("psf", po)])
                                if kt == nkt - 1:
                                    r = (hh + qb) % 2
                                    S.op("act", lambda e: e.activation(out=rec[r][:], in_=psf[po][64:128, :], func=AF.Ln), reads=[("psf", po)], writes=[("rec", r)])
                                    S.op("act", lambda e: e.activation(out=rec[r][:], in_=rec[r][:], func=AF.Exp, scale=-1.0), reads=[("rec", r)], writes=[("rec", r)])
                                    S.op("dve", lambda e: e.tensor_tensor(
                                        out=OT[hh * 64:(hh + 1) * 64, hp, qb * 512:(qb + 1) * 512], in0=psf[po][0:64, :], in1=rec[r][:], op=ALU.mult),
                                        reads=[("psf", po), ("rec", r)], writes=[("OT", qb * 4 + i) for i in range(4)])

                            apipe.push(front, back)
                apipe.drain()
            if os.environ.get("MK_WO", "1") == "1":
                add_proj_to_h(OT, "OT", woa, "woa", KC)
            S.flush()
        st.close()

    def phase_ffn():
        with ExitStack() as st:
            xnT = sb("xnT", [128, KC, T], BF16, st)
            load_gain(norm_ffn[0:1, :])
            norm_stats()
            norm_to_T(xnT, "xnT")
            swiglu_block(xnT, "xnT", ffn_wg, ffn_wu, ffn_wd, 2816, st)
            S.flush()

    def phase_dsw():
        with ExitStack() as st:
            hnT = sb("hnT", [128, KC, T], BF16, st)
            gq = sb("gq1", [128, KC], F32, st)
            gk = sb("gk1", [128, KC], F32, st)
            wq = [sb("wq%d" % i, [128, KC, 384], BF16, st) for i in range(1)]
            wk = [sb("wk%d" % i, [128, KC, 384], BF16, st) for i in range(1)]
            wv = [sb("wv%d" % i, [128, KC, 384], BF16, st) for i in range(1)]
            wo2 = [sb("wo2%d" % i, [128, 1, D], BF16, st) for i in range(2)]
            OTp = [sb("OTp%d" % i, [128, 1, T], BF16, st) for i in range(2)]
            qtok = [sb("qtk%d" % i, [128, 6, 64], BF16, st) for i in range(2)]
            ktok = [sb("ktk%d" % i, [128, 6, 64], BF16, st) for i in range(2)]
            rtq = [sb("rtq%d" % i, [128, 4, 6, 8], F32, st) for i in range(1)]
            rtk = [sb("rtk%d" % i, [128, 4, 6, 8], F32, st) for i in range(1)]
            QT = sb("QTb", [128, 3, T], BF16, st)
            KT = sb("KTb", [128, 3, T], BF16, st)
            VA = sb("VAb", [128, 3, NT, 2, 128], BF16, st)
            PT = [sb("PTb%d" % i, [128, 512], BF16, st) for i in range(4)]
            rec = [sb("recb%d" % i, [64, 512], F32, st) for i in range(2)]
            dma_sp(gq[:], norm_attn[1:2, :].rearrange("o (c p) -> p (o c)", p=128), "gq1", writes=["gq1"], slow=True)
            dma_sp(gk[:], dsw_kv_norm.rearrange("o (c p) -> p (o c)", p=128), "gk1", writes=["gk1"], slow=True)
            norm_stats()
            norm_to_T(hnT, "hnT", use_gain=False)
            S.op("dve", lambda e: e.memset(VA[:], 1.0), writes=["VA1"])
            wqv = w_q.rearrange("(c p) n -> p c n", p=128)
            wkvv = w_kv.rearrange("(c p) n -> p c n", p=128)

            def load_pair(hp):
                s = 0
                dma_cast(wo2[hp % 2][:, 0, :], w_o_b[hp * 128:(hp + 1) * 128, :], ("wo2", hp % 2), writes=[("wo2", hp % 2)])
                for g in range(3):
                    for c in range(KC):
                        dma_cast(wq[s][:, c, g * 128:(g + 1) * 128], wqv[:, c, g * 1024 + hp * 128: g * 1024 + (hp + 1) * 128], ("wq", s), writes=[("wq", s)])
                for g in range(3):
                    for c in range(KC):
                        dma_cast(wk[s][:, c, g * 128:(g + 1) * 128], wkvv[:, c, g * 1024 + hp * 128: g * 1024 + (hp + 1) * 128], ("wk", s), writes=[("wk", s)])
                for g in range(3):
                    for c in range(KC):
                        dma_cast(wv[s][:, c, g * 128:(g + 1) * 128], wkvv[:, c, 3072 + g * 1024 + hp * 128: 3072 + g * 1024 + (hp + 1) * 128], ("wv", s), writes=[("wv", s)])
                for c in range(KC):
                    S.op("dve", lambda e, s=s, c=c: e.tensor_scalar(out=wq[s][:, c, :], in0=wq[s][:, c, :], scalar1=gq[:, c:c + 1], scalar2=None, op0=ALU.mult),
                         reads=[("wq", s), "gq1"], writes=[("wq", s)])
                    S.op("dve", lambda e, s=s, c=c: e.tensor_scalar(out=wk[s][:, c, :], in0=wk[s][:, c, :], scalar1=gk[:, c:c + 1], scalar2=None, op0=ALU.mult),
                         reads=[("wk", s), "gk1"], writes=[("wk", s)])
                    S.op("dve", lambda e, s=s, c=c: e.tensor_scalar(out=wv[s][:, c, :], in0=wv[s][:, c, :], scalar1=gk[:, c:c + 1], scalar2=None, op0=ALU.mult),
                         reads=[("wv", s), "gk1"], writes=[("wv", s)])

            def rope6(ps3, dst, rt, b, t, pkey, dkey, rkey):
                co = cs_dsw[:, t, 0:1, :].to_broadcast([128, 6, 8])
                si = cs_dsw[:, t, 1:2, :].to_broadcast([128, 6, 8])
                x1 = ps3[:, :, 0:8]
                x2 = ps3[:, :, 8:16]
                S.op("act", lambda e: e.copy(out=dst[:, :, 16:64], in_=ps3[:, :, 16:64]), reads=[pkey], writes=[dkey])
                S.op("dve", lambda e: e.tensor_tensor(out=rt[:, 0], in0=x1, in1=co, op=ALU.mult), reads=[pkey, ("cs_dsw", t, 0)], writes=[rkey])
                S.op("dve", lambda e: e.tensor_tensor(out=rt[:, 1], in0=x2, in1=si, op=ALU.mult), reads=[pkey, ("cs_dsw", t, 1)], writes=[rkey])
                S.op("dve", lambda e: e.tensor_tensor(out=rt[:, 2], in0=x2, in1=co, op=ALU.mult), reads=[pkey, ("cs_dsw", t, 0)], writes=[rkey])
                S.op("dve", lambda e: e.tensor_tensor(out=rt[:, 3], in0=x1, in1=si, op=ALU.mult), reads=[pkey, ("cs_dsw", t, 1)], writes=[rkey])
                S.op("dve", lambda e: e.tensor_tensor(out=dst[:, :, 0:8], in0=rt[:, 0], in1=rt[:, 1], op=ALU.subtract), reads=[rkey], writes=[dkey])
                S.op("dve", lambda e: e.tensor_tensor(out=dst[:, :, 8:16], in0=rt[:, 2], in1=rt[:, 3], op=ALU.add), reads=[rkey], writes=[dkey])

            c6d = [0]

            def next6():
                i = c6d[0] % 6
                c6d[0] += 1
                return i

            load_pair(0)
            npt = 0
            sc = 1.0 / 8.0
            DIL = (1, 4, 16)
            MASK = {1: (0, None, 1), 4: (2, 3, 4), 16: (5, 6, None)}
            for hp in range(int(os.environ.get("MK_HP", "8"))):
                s = 0
                tpipe = Pipe(1)
                for t in range(NT):
                  def front_t(t=t, s=s):
                    b = t % 2
                    pq = next6()
                    pk = next6()
                    pv = next6()
                    for (pp, ww, wkey) in ((pq, wq, "wq"), (pk, wk, "wk"), (pv, wv, "wv")):
                        for k in range(KC):
                            S.op("pe", lambda e, k=k, t=t, pp=pp, ww=ww, s=s: e.matmul(psf[pp][:, 0:384], hnT[:, k, t * 128:(t + 1) * 128], ww[s][:, k, :], start=(k == 0), stop=(k == KC - 1)),
                                 reads=[("hnT", t), (wkey, s)], writes=[("psf", pp)])
                    q3 = psf[pq][:, 0:384].rearrange("p (s d) -> p s d", s=6)
                    k3 = psf[pk][:, 0:384].rearrange("p (s d) -> p s d", s=6)
                    rope6(q3, qtok[b], rtq[0], b, t, ("psf", pq), ("qtk", b), ("rtq", 0))
                    rope6(k3, ktok[b], rtk[0], b, t, ("psf", pk), ("ktk", b), ("rtk", 0))
                    S.op("act", lambda e, t=t, pv=pv: e.copy(out=VA[:, :, t, :, 0:64], in_=psf[pv][:, 0:384].rearrange("p (g h d) -> p g h d", g=3, h=2)),
                         reads=[("psf", pv), "VA1"], writes=[("VAb", t)])
                  def back_t(t=t):
                    b = t % 2
                    pb = next_psb()
                    for g in range(3):
                        S.op("pe", lambda e, g=g, b=b, pb=pb: e.transpose(psb[pb][:, g * 128:(g + 1) * 128], qtok[b][:, 2 * g:2 * g + 2, :].rearrange("p a d -> p (a d)"), ident_b[:]),
                             reads=[("qtk", b), "ident_b"], writes=[("psb", pb)])
                    for g in range(3):
                        S.op("pe", lambda e, g=g, b=b, pb=pb: e.transpose(psb[pb][:, 384 + g * 128:384 + (g + 1) * 128], ktok[b][:, 2 * g:2 * g + 2, :].rearrange("p a d -> p (a d)"), ident_b[:]),
                             reads=[("ktk", b), "ident_b"], writes=[("psb", pb)])
                    S.op("act", lambda e, t=t, pb=pb: e.copy(out=QT[:, :, t * 128:(t + 1) * 128], in_=psb[pb][:, 0:384].rearrange("p (g c) -> p g c", g=3)),
                         reads=[("psb", pb)], writes=[("QTb", t)])
                    S.op("act", lambda e, t=t, pb=pb: e.copy(out=KT[:, :, t * 128:(t + 1) * 128], in_=psb[pb][:, 384:768].rearrange("p (g c) -> p g c", g=3)),
                         reads=[("psb", pb)], writes=[("KTb", t)])
                  tpipe.push(front_t, back_t)
                tpipe.drain()
                if hp + 1 < 8:
                    load_pair(hp + 1)
                apipe = Pipe(3)
                for hh in range(2):
                    r0 = hh * 64
                    for qb in range(4):
                        po = next_pso()
                        contribs = []
                        for g in (2, 1, 0):
                            d = DIL[g]
                            for kt in range(0, 4 * qb + 4):
                                lo = max(kt, 4 * qb)
                                hi = min(kt + d, 4 * qb + 3)
                                if lo > hi:
                                    continue
                                contribs.append((g, kt, lo, hi))
                        assert contribs[0][2] == 4 * qb and contribs[0][3] == 4 * qb + 3
                        for ci, (g, kt, lo, hi) in enumerate(contribs):
                            d = DIL[g]
                            c0 = (lo - 4 * qb) * 128
                            c1 = (hi - 4 * qb + 1) * 128
                            psi = next_psf()
                            pt = npt % 4
                            npt += 1
                            segs = []
                            for qt in range(lo, hi + 1):
                                if qt == kt:
                                    mi = MASK[d][0]
                                elif qt - kt == d:
                                    mi = MASK[d][2]
                                else:
                                    mi = MASK[d][1]
                                a0 = (qt - 4 * qb) * 128
                                if mi is None:
                                    continue
                                if segs and segs[-1][0] == mi and mi == MASK[d][1] and segs[-1][2] == a0:
                                    segs[-1][2] = a0 + 128
                                else:
                                    segs.append([mi, a0, a0 + 128])

                            def front(g=g, kt=kt, c0=c0, c1=c1, psi=psi, r0=r0, qb=qb, segs=segs, pt=pt):
                                S.op("pe", lambda e: e.matmul(
                                    psf[psi][:, c0:c1], KT[r0:r0 + 64, g, kt * 128:(kt + 1) * 128], QT[r0:r0 + 64, g, qb * 512 + c0:qb * 512 + c1], start=True, stop=True),
                                    reads=[("KTb", kt)] + [("QTb", qb * 4 + i) for i in range(4)], writes=[("psf", psi)])
                                S.op("act", lambda e: e.activation(out=PT[pt][:, c0:c1], in_=psf[psi][:, c0:c1], func=AF.Exp, scale=sc),
                                     reads=[("psf", psi)], writes=[("PTb", pt)])
                                for si_, (mi, a0, a1) in enumerate(segs):
                                    S.op("dve", lambda e, mi=mi, a0=a0, a1=a1: e.tensor_tensor(out=PT[pt][:, a0:a1], in0=PT[pt][:, a0:a1], in1=masks[:, mi, 0:a1 - a0], op=ALU.mult),
                                         reads=[("PTb", pt), "masks"], writes=[("PTb", pt)])

                            def back(g=g, kt=kt, hh=hh, c0=c0, c1=c1, po=po, pt=pt, ci=ci, n=len(contribs), qb=qb, hp=hp):
                                S.op("pe", lambda e: e.matmul(
                                    psf[po][:, c0:c1], VA[:, g, kt, hh, :], PT[pt][:, c0:c1], start=(ci == 0), stop=(ci == n - 1)),
                                    reads=[("VAb", kt), "VA1", ("PTb", pt)], writes=[("psf", po)])
                                if ci == n - 1:
                                    r = (hh + qb) % 2
                                    S.op("act", lambda e: e.activation(out=rec[r][:], in_=psf[po][64:128, :], func=AF.Ln), reads=[("psf", po)], writes=[("recb", r)])
                                    S.op("act", lambda e: e.activation(out=rec[r][:], in_=rec[r][:], func=AF.Exp, scale=-1.0), reads=[("recb", r)], writes=[("recb", r)])
                                    S.op("dve", lambda e: e.tensor_tensor(
                                        out=OTp[hp % 2][hh * 64:(hh + 1) * 64, 0, qb * 512:(qb + 1) * 512], in0=psf[po][0:64, :], in1=rec[r][:], op=ALU.mult),
                                        reads=[("psf", po), ("recb", r)], writes=[(("OTp", hp % 2), qb * 4 + i) for i in range(4)])

                            apipe.push(front, back)
                apipe.drain()
                add_proj_to_h(OTp[hp % 2], ("OTp", hp % 2), wo2[hp % 2], ("wo2", hp % 2), 1)
            S.flush()

    def phase_moe_dense():
        with ExitStack() as st:
            xnT = sb("xnT", [128, KC, T], BF16, st)
            comb = sb("comb", [128, NT, 8], F32, st)
            rtr = sb("rtr", [128, KC, 8], F32, st)
            xnf = [sb("xnf%d" % i, [128, D], F32, st) for i in range(2)]
            xTf = [sb("xTf%d" % i, [128, D], F32, st) for i in range(2)]
            lg = sb("lg", [128, NT, 8], F32, st)
            m1 = sb("m1", [128, NT, 4], F32, st)
            tmp8 = sb("tmp8", [128, NT, 3, 8], F32, st)
            load_gain(norm_ffn[1:2, :])
            dma_sp(rtr[:], router.rearrange("(c p) e -> p c e", p=128), "rtr", writes=["rtr"])
            norm_stats()
            for t in range(NT):
                b = t % 2
                S.op("dve", lambda e, t=t, b=b: e.scalar_tensor_tensor(out=xnf[b][:], in0=h[:, t, :], scalar=rstd[:, t:t + 1], in1=gbc_ref[0][:], op0=ALU.mult, op1=ALU.mult),
                     reads=[("h", t), ("rstd", t), "gbc"], writes=[("xnf", b)])
                pa = next_psf()
                pb2 = next_psf()
                for k in range(KC):
                    pp = pa if k < 4 else pb2
                    S.op("pe", lambda e, k=k, b=b, pp=pp: e.transpose(psf[pp][:, (k % 4) * 128:(k % 4 + 1) * 128], xnf[b][:, k * 128:(k + 1) * 128], ident_f[:]),
                         reads=[("xnf", b), "ident_f"], writes=[("psf", pp)])
                S.op("act", lambda e, b=b, pa=pa: e.copy(out=xTf[b][:, 0:512], in_=psf[pa][:]), reads=[("psf", pa)], writes=[("xTf", b, 0)])
                S.op("act", lambda e, b=b, pb2=pb2: e.copy(out=xTf[b][:, 512:1024], in_=psf[pb2][:]), reads=[("psf", pb2)], writes=[("xTf", b, 1)])
                pl = next_psf()
                for k in range(KC):
                    S.op("pe", lambda e, k=k, b=b, pl=pl: e.matmul(psf[pl][:, 0:8], xTf[b][:, k * 128:(k + 1) * 128], rtr[:, k, :], start=(k == 0), stop=(k == KC - 1)),
                         reads=[("xTf", b, 0), ("xTf", b, 1), "rtr"], writes=[("psf", pl)])
                S.op("dve", lambda e, t=t, pl=pl: e.tensor_copy(out=lg[:, t, :], in_=psf[pl][:, 0:8]), reads=[("psf", pl)], writes=[("lg", t)])
                L = lg[:, t, :]
                S.op("dve", lambda e, t=t, L=L: e.reduce_max(out=m1[:, t, 0:1], in_=L, axis=AX.X), reads=[("lg", t)], writes=[("m1", t)])
                S.op("dve", lambda e, t=t, L=L: e.tensor_scalar(out=tmp8[:, t, 0, :], in0=L, scalar1=m1[:, t, 0:1], scalar2=-1e30, op0=ALU.is_ge, op1=ALU.mult),
                     reads=[("lg", t), ("m1", t)], writes=[("tmp8", t)])
                S.op("dve", lambda e, t=t, L=L: e.tensor_tensor(out=tmp8[:, t, 0, :], in0=tmp8[:, t, 0, :], in1=L, op=ALU.add), reads=[("tmp8", t), ("lg", t)], writes=[("tmp8", t)])
                S.op("dve", lambda e, t=t: e.reduce_max(out=m1[:, t, 1:2], in_=tmp8[:, t, 0, :], axis=AX.X), reads=[("tmp8", t)], writes=[("m1", t)])
                S.op("dve", lambda e, t=t, L=L: e.tensor_scalar(out=tmp8[:, t, 1, :], in0=L, scalar1=m1[:, t, 1:2], scalar2=None, op0=ALU.is_ge),
                     reads=[("lg", t), ("m1", t)], writes=[("tmp8", t)])
                S.op("dve", lambda e, t=t, L=L: e.tensor_scalar(out=tmp8[:, t, 2, :], in0=L, scalar1=m1[:, t, 0:1], scalar2=None, op0=ALU.subtract),
                     reads=[("lg", t), ("m1", t)], writes=[("tmp8", t)])
                S.op("act", lambda e, t=t: e.activation(out=tmp8[:, t, 2, :], in_=tmp8[:, t, 2, :], func=AF.Exp), reads=[("tmp8", t)], writes=[("tmp8", t)])
                S.op("dve", lambda e, t=t: e.tensor_tensor(out=tmp8[:, t, 2, :], in0=tmp8[:, t, 2, :], in1=tmp8[:, t, 1, :], op=ALU.mult), reads=[("tmp8", t)], writes=[("tmp8", t)])
                S.op("dve", lambda e, t=t: e.reduce_sum(out=m1[:, t, 2:3], in_=tmp8[:, t, 2, :], axis=AX.X), reads=[("tmp8", t)], writes=[("m1", t)])
                S.op("dve", lambda e, t=t: e.reciprocal(out=m1[:, t, 3:4], in_=m1[:, t, 2:3]), reads=[("m1", t)], writes=[("m1", t)])
                S.op("dve", lambda e, t=t: e.tensor_scalar(out=comb[:, t, :], in0=tmp8[:, t, 2, :], scalar1=m1[:, t, 3:4], scalar2=None, op0=ALU.mult),
                     reads=[("tmp8", t), ("m1", t)], writes=[("comb", t)])
            norm_to_T(xnT, "xnT")
            S.flush()
            for ex in range(8):
                with ExitStack() as st2:
                    swiglu_block(xnT, "xnT", moe_wg[ex], moe_wu[ex], moe_wd[ex], 3584, st2,
                                 scale_col=lambda t, ex=ex: (comb[:, t, ex:ex + 1], ("comb", t)))
                    S.flush()

    def phase_moe():
        es_attn.close()
        c6 = [0]

        def next6():
            i = c6[0] % 6
            c6[0] += 1
            return i

        with ExitStack() as st:
            xn_tok = sb("xn_tok", [128, NT, D], BF16, st)
            comb = sb("comb", [128, NT, 8], F32, st)
            comb_hl = sb("comb_hl", [128, NT, 8, 2], BF16, st)
            comb_ov = sb("comb_ov", [128, NT, 8], F32, st)
            pos = sb("pos", [128, NT, 8], F32, st)
            iota = sb("iota", [128, CAP], F32, st)
            with ExitStack() as st1:
                gbc_ref[0] = sb("gbc_r", [128, D], F32, st1)
                rtr = sb("rtr", [128, KC, 8], F32, st1)
                xnf = [sb("xnf%d" % i, [128, D], F32, st1) for i in range(2)]
                xTf = [sb("xTf%d" % i, [128, D], F32, st1) for i in range(2)]
                lg = sb("lg", [128, NT, 8], F32, st1)
                m1 = sb("m1", [128, NT, 4], F32, st1)
                tmp8 = sb("tmp8", [128, NT, 3, 8], F32, st1)
                selb = sb("selb", [128, NT, 8], BF16, st1)
                ltri = sb("ltri", [128, 128], BF16, st1)
                ones_b = sb("ones_b", [128, 128], BF16, st1)
                tot = sb("tot", [128, NT, 8], F32, st1)
                cum = sb("cum", [128, NT, 8], F32, st1)
                chi = sb("chi", [128, NT, 8], F32, st1)
                ne = sb("ne", [128, 8], F32, st1)
                flag_f = sb("flag_f", [128, 8], F32, st1)
                flag_i = sb("flag_i", [128, 8], I32, st1)
                load_gain(norm_ffn[1:2, :])
                dma_sp(rtr[:], router.rearrange("(c p) e -> p c e", p=128), "rtr", writes=["rtr"])
                dma_sp(iota[:], c_iota, "iota", writes=["iota"])
                dma_cast(ltri[:], c_ltri, "ltri", writes=["ltri"])
                S.op("dve", lambda e: e.memset(ones_b[:], 1.0), writes=["ones_b"])
                norm_stats()
                for t in range(NT):
                    b = t % 2
                    S.op("dve", lambda e, t=t, b=b: e.scalar_tensor_tensor(out=xnf[b][:], in0=h[:, t, :], scalar=rstd[:, t:t + 1], in1=gbc_ref[0][:], op0=ALU.mult, op1=ALU.mult),
                         reads=[("h", t), ("rstd", t), "gbc"], writes=[("xnf", b)])
                    S.op("act", lambda e, t=t, b=b: e.copy(out=xn_tok[:, t, :], in_=xnf[b][:]), reads=[("xnf", b)], writes=[("xn_tok", t)])
                    pa = next_psf()
                    pb2 = next_psf()
                    for k in range(KC):
                        pp = pa if k < 4 else pb2
                        S.op("pe", lambda e, k=k, b=b, pp=pp: e.transpose(psf[pp][:, (k % 4) * 128:(k % 4 + 1) * 128], xnf[b][:, k * 128:(k + 1) * 128], ident_f[:]),
                             reads=[("xnf", b), "ident_f"], writes=[("psf", pp)])
                    S.op("act", lambda e, b=b, pa=pa: e.copy(out=xTf[b][:, 0:512], in_=psf[pa][:]), reads=[("psf", pa)], writes=[("xTf", b, 0)])
                    S.op("act", lambda e, b=b, pb2=pb2: e.copy(out=xTf[b][:, 512:1024], in_=psf[pb2][:]), reads=[("psf", pb2)], writes=[("xTf", b, 1)])
                    pl = next_psf()
                    for k in range(KC):
                        S.op("pe", lambda e, k=k, b=b, pl=pl: e.matmul(psf[pl][:, 0:8], xTf[b][:, k * 128:(k + 1) * 128], rtr[:, k, :], start=(k == 0), stop=(k == KC - 1)),
                             reads=[("xTf", b, 0), ("xTf", b, 1), "rtr"], writes=[("psf", pl)])
                    S.op("dve", lambda e, t=t, pl=pl: e.tensor_copy(out=lg[:, t, :], in_=psf[pl][:, 0:8]), reads=[("psf", pl)], writes=[("lg", t)])
                allt = lambda nm: [(nm, t) for t in range(NT)]
                bc = lambda col: m1[:, :, col:col + 1].to_broadcast([128, NT, 8])
                T0, T1, T2 = tmp8[:, :, 0, :], tmp8[:, :, 1, :], tmp8[:, :, 2, :]
                S.op("dve", lambda e: e.reduce_max(out=m1[:, :, 0], in_=lg[:], axis=AX.X), reads=allt("lg"), writes=["m1a"])
                S.op("dve", lambda e: e.tensor_tensor(out=T0, in0=lg[:], in1=bc(0), op=ALU.is_ge), reads=allt("lg") + ["m1a"], writes=["T0"])
                S.op("dve", lambda e: e.scalar_tensor_tensor(out=T0, in0=T0, scalar=-1e30, in1=lg[:], op0=ALU.mult, op1=ALU.add), reads=allt("lg") + ["T0"], writes=["T0"])
                S.op("dve", lambda e: e.reduce_max(out=m1[:, :, 1], in_=T0, axis=AX.X), reads=["T0"], writes=["m1b"])
                S.op("dve", lambda e: e.tensor_tensor(out=T1, in0=lg[:], in1=bc(1), op=ALU.is_ge), reads=allt("lg") + ["m1b"], writes=allt("tmp8"))
                S.op("dve", lambda e: e.tensor_tensor(out=T2, in0=lg[:], in1=bc(0), op=ALU.subtract), reads=allt("lg") + ["m1a"], writes=["T2"])
                S.op("act", lambda e: e.activation(out=T2, in_=T2, func=AF.Exp), reads=["T2"], writes=["T2"])
                S.op("dve", lambda e: e.tensor_tensor(out=T2, in0=T2, in1=T1, op=ALU.mult), reads=["T2"] + allt("tmp8"), writes=["T2"])
                S.op("dve", lambda e: e.reduce_sum(out=m1[:, :, 2], in_=T2, axis=AX.X), reads=["T2"], writes=["m1c"])
                S.op("dve", lambda e: e.reciprocal(out=m1[:, :, 3], in_=m1[:, :, 2]), reads=["m1c"], writes=["m1d"])
                S.op("dve", lambda e: e.tensor_tensor(out=comb[:], in0=T2, in1=bc(3), op=ALU.mult), reads=["T2", "m1d"], writes=allt("comb"))
                S.op("dve", lambda e: e.tensor_copy(out=selb[:], in_=T1), reads=allt("tmp8"), writes=allt("selb"))
                S.op("pe", lambda e: e.matmul(psf[0][:, 0:128], ltri[:], selb[:].rearrange("p t e -> p (t e)"), start=True, stop=True), reads=["ltri"] + allt("selb"), writes=[("psf", 0)])
                S.op("pe", lambda e: e.matmul(psf[1][:, 0:128], ones_b[:], selb[:].rearrange("p t e -> p (t e)"), start=True, stop=True), reads=["ones_b"] + allt("selb"), writes=[("psf", 1)])
                S.op("dve", lambda e: e.tensor_copy(out=tot[:].rearrange("p t e -> p (t e)"), in_=psf[1][:, 0:128]), reads=[("psf", 1)], writes=["tot"])
                S.op("dve", lambda e: e.memset(cum[:, 0, :], 0.0), writes=[("cum", 0)])
                for t in range(1, NT):
                    S.op("dve", lambda e, t=t: e.tensor_tensor(out=cum[:, t, :], in0=cum[:, t - 1, :], in1=tot[:, t - 1, :], op=ALU.add), reads=[("cum", t - 1), "tot"], writes=[("cum", t)])
                S.op("dve", lambda e: e.tensor_tensor(out=pos[:].rearrange("p t e -> p (t e)"), in0=psf[0][:, 0:128], in1=cum[:].rearrange("p t e -> p (t e)"), op=ALU.add),
                     reads=[("psf", 0)] + allt("cum"), writes=["pos"])
                S.op("dve", lambda e: e.scalar_tensor_tensor(out=pos[:], in0=pos[:], scalar=1.0, in1=tmp8[:, :, 1, :], op0=ALU.add, op1=ALU.mult), reads=["pos"] + allt("tmp8"), writes=["pos"])
                S.op("dve", lambda e: e.tensor_scalar(out=pos[:], in0=pos[:], scalar1=-1.0, scalar2=None, op0=ALU.add), reads=["pos"], writes=["pos"])
                S.op("dve", lambda e: e.scalar_tensor_tensor(out=comb_ov[:], in0=pos[:], scalar=float(CAP), in1=comb[:], op0=ALU.is_ge, op1=ALU.mult), reads=["pos"] + allt("comb"), writes=["comb_ov"])
                S.op("dve", lambda e: e.tensor_tensor(out=ne[:], in0=cum[:, NT - 1, :], in1=tot[:, NT - 1, :], op=ALU.add), reads=[("cum", NT - 1), "tot"], writes=["ne"])
                S.op("dve", lambda e: e.reduce_max(out=flag_f[:, 0:1], in_=ne[:], axis=AX.X), reads=["ne"], writes=["flag_f0"])
                S.op("dve", lambda e: e.tensor_scalar(out=flag_f[:, 1:2], in0=flag_f[:, 0:1], scalar1=float(CAP), scalar2=None, op0=ALU.is_gt), reads=["flag_f0"], writes=["flag_f1"])
                S.op("dve", lambda e: e.tensor_copy(out=flag_i[:, 0:8], in_=flag_f[:, 1:2].to_broadcast([128, 8])), reads=["flag_f1"], writes=["flag_i"])
                dma_sp(flag_d, flag_i[0:1, :], "flag", reads=["flag_i"])
                S.op("dve", lambda e: e.tensor_copy(out=comb_hl[:, :, :, 0], in_=comb[:]), reads=allt("comb"), writes=["comb_h"])
                S.op("dve", lambda e: e.tensor_copy(out=chi[:], in_=comb_hl[:, :, :, 0]), reads=["comb_h"], writes=["chi"])
                S.op("dve", lambda e: e.tensor_tensor(out=comb_hl[:, :, :, 1], in0=comb[:], in1=chi[:], op=ALU.subtract), reads=["chi"] + allt("comb"), writes=["comb_l"])
                S.flush()
            with ExitStack() as st2:
                Sel = [sb("Sel%d" % i, [128, NT, 128], BF16, st2) for i in range(2)]
                GT = sb("GT", [128, NJ, T], BF16, st2)
                XY = sb("XY", [128, 8 * CAP], BF16, st2)
                XeT = XY[:, :].rearrange("p (k c) -> p k c", k=8)
                Yv = XY[:, :].rearrange("p (j d) -> p j d", j=NJ)
                hT = sb("hTs", [128, 28, CAP], BF16, st2)
                wg2 = [sb("wg2%d" % i, [128, KC, 256], BF16, st2) for i in range(3)]
                wu2 = [sb("wu2%d" % i, [128, KC, 256], BF16, st2) for i in range(3)]
                wd2 = [sb("wd2%d" % i, [128, 2, 512], BF16, st2) for i in range(2)]
                sil = [sb("sils%d" % i, [128, CAP // 2], F32, st2) for i in range(2)]
                gsl = sb("gsl", [128, 2 * NJ], F32, st2)
                gate_e = sb("gate_e", [128, NJ], F32, st2)
                HW_ = CAP // 2
                n_gu = 8 * 14
                n_d = 8 * 2 * 14

                def issue_gu(i):
                    ex, grp = divmod(i, 14)
                    s_ = i % 3
                    wgv = moe_wg[ex].rearrange("(c p) n -> p c n", p=128)
                    wuv = moe_wu[ex].rearrange("(c p) n -> p c n", p=128)
                    for c in range(KC):
                        dma_cast(wg2[s_][:, c, :], wgv[:, c, grp * 256:(grp + 1) * 256], ("wg2", s_), writes=[("wg2", s_)])
                    for c in range(KC):
                        dma_cast(wu2[s_][:, c, :], wuv[:, c, grp * 256:(grp + 1) * 256], ("wu2", s_), writes=[("wu2", s_)])

                def issue_d(i):
                    ex, r = divmod(i, 28)
                    oh, g2 = divmod(r, 14)
                    s_ = i % 2
                    wdv = moe_wd[ex].rearrange("(c p) n -> p c n", p=128)
                    for c in range(2):
                        dma_cast(wd2[s_][:, c, :], wdv[:, g2 * 2 + c, oh * 512:(oh + 1) * 512], ("wd2", s_), writes=[("wd2", s_)])

                issue_gu(0)
                issue_gu(1)
                issue_d(0)
                nsil = 0
                allXY = [("XY", j) for j in range(NJ)]
                for ex in range(8):
                    for j in range(NJ):
                        sl = j % 2
                        for t in range(j, NT):
                            S.op("dve", lambda e, j=j, t=t, sl=sl, ex=ex: e.tensor_scalar(out=Sel[sl][:, t, :], in0=iota[:, j * 128:(j + 1) * 128], scalar1=pos[:, t, ex:ex + 1], scalar2=None, op0=ALU.is_equal),
                                 reads=["pos", "iota"], writes=[("Sel", sl, t)])
                        ba, bb = (0, 1) if j % 2 == 0 else (2, 3)
                        for k in range(KC):
                            bank = ba if k < 4 else bb
                            for t in range(j, NT):
                                S.op("pe", lambda e, k=k, t=t, sl=sl, bank=bank, j=j: e.matmul(psf[bank][:, (k % 4) * 128:(k % 4 + 1) * 128], xn_tok[:, t, k * 128:(k + 1) * 128], Sel[sl][:, t, :], start=(t == j), stop=(t == NT - 1)),
                                     reads=[("xn_tok", t), ("Sel", sl, t)], writes=[("psf", bank)])
                        S.op("act", lambda e, j=j, ba=ba: e.copy(out=XeT[:, 0:4, j * 128:(j + 1) * 128], in_=psf[ba][:].rearrange("p (k c) -> p k c", k=4)), reads=[("psf", ba)], writes=[("XY", j)])
                        S.op("act", lambda e, j=j, bb=bb: e.copy(out=XeT[:, 4:8, j * 128:(j + 1) * 128], in_=psf[bb][:].rearrange("p (k c) -> p k c", k=4)), reads=[("psf", bb)], writes=[("XY", j)])
                        for t in range(j, NT):
                            S.op("pe", lambda e, j=j, t=t, sl=sl, ex=ex: e.matmul(psf[4][:, 2 * j:2 * j + 2], Sel[sl][:, t, :], comb_hl[:, t, ex, :], start=(t == j), stop=(t == NT - 1)),
                                 reads=[("Sel", sl, t), "comb_h", "comb_l"], writes=[("psf", 4)])
                        for t in range(j, NT):
                            pb = 0 if t < 8 else 1
                            S.op("pe", lambda e, t=t, sl=sl, pb=pb: e.transpose(psb[pb][:, (t % 8) * 128:(t % 8 + 1) * 128], Sel[sl][:, t, :], ident_b[:]),
                                 reads=[("Sel", sl, t), "ident_b"], writes=[("psb", pb)])
                        S.op("dve", lambda e, j=j: e.tensor_copy(out=GT[:, j, j * 128:1024], in_=psb[0][:, j * 128:1024]), reads=[("psb", 0)], writes=[("GT", j)])
                        S.op("dve", lambda e, j=j: e.tensor_copy(out=GT[:, j, 1024:2048], in_=psb[1][:]), reads=[("psb", 1)], writes=[("GT", j)])
                    S.op("dve", lambda e: e.tensor_copy(out=gsl[:], in_=psf[4][:, 0:2 * NJ]), reads=[("psf", 4)], writes=["gsl"])
                    g2v = gsl[:, :].rearrange("p (j two) -> p j two", two=2)
                    S.op("dve", lambda e, g2v=g2v: e.tensor_tensor(out=gate_e[:], in0=g2v[:, :, 0], in1=g2v[:, :, 1], op=ALU.add), reads=["gsl"], writes=["gate_e"])
                    for grp in range(14):
                        i = ex * 14 + grp
                        s_ = i % 3
                        if i + 2 < n_gu:
                            issue_gu(i + 2)
                        for fc in range(2):
                            c = grp * 2 + fc
                            for half in range(2):
                                pg = next6()
                                pu = next6()
                                for k in range(KC):
                                    S.op("pe", lambda e, k=k, fc=fc, half=half, pg=pg, s_=s_: e.matmul(psf[pg][:, 0:HW_], wg2[s_][:, k, fc * 128:(fc + 1) * 128], XeT[:, k, half * HW_:(half + 1) * HW_], start=(k == 0), stop=(k == KC - 1)),
                                         reads=[("wg2", s_)] + allXY, writes=[("psf", pg)])
                                for k in range(KC):
                                    S.op("pe", lambda e, k=k, fc=fc, half=half, pu=pu, s_=s_: e.matmul(psf[pu][:, 0:HW_], wu2[s_][:, k, fc * 128:(fc + 1) * 128], XeT[:, k, half * HW_:(half + 1) * HW_], start=(k == 0), stop=(k == KC - 1)),
                                         reads=[("wu2", s_)] + allXY, writes=[("psf", pu)])
                                si = nsil % 2
                                nsil += 1
                                S.op("act", lambda e, pg=pg, si=si: e.activation(out=sil[si][:], in_=psf[pg][:, 0:HW_], func=AF.Silu), reads=[("psf", pg)], writes=[("sils", si)])
                                S.op("dve", lambda e, pu=pu, si=si, c=c, half=half: e.tensor_tensor(out=hT[:, c, half * HW_:(half + 1) * HW_], in0=psf[pu][:, 0:HW_], in1=sil[si][:], op=ALU.mult),
                                     reads=[("psf", pu), ("sils", si)], writes=[("hTs", c)])
                    for oh in range(2):
                        for g2 in range(14):
                            i = (ex * 2 + oh) * 14 + g2
                            s_ = i % 2
                            if i + 1 < n_d:
                                issue_d(i + 1)
                            for j in range(NJ):
                                for ci in range(2):
                                    c = g2 * 2 + ci
                                    S.op("pe", lambda e, j=j, ci=ci, c=c, s_=s_: e.matmul(psf[j][:], hT[:, c, j * 128:(j + 1) * 128], wd2[s_][:, ci, :], start=(c == 0), stop=(c == 27)),
                                         reads=[("hTs", c), ("wd2", s_)], writes=[("psf", j)])
                        for j in range(NJ):
                            S.op("dve", lambda e, j=j, oh=oh: e.tensor_scalar(out=Yv[:, j, oh * 512:(oh + 1) * 512], in0=psf[j][:], scalar1=gate_e[:, j:j + 1], scalar2=None, op0=ALU.mult),
                                 reads=[("psf", j), "gate_e"], writes=[("XY", j)])
                    for t in range(NT):
                        for half in range(2):
                            b_ = next6()
                            jmax = min(t, NJ - 1)
                            for j in range(jmax + 1):
                                S.op("pe", lambda e, j=j, t=t, half=half, b_=b_, jmax=jmax: e.matmul(psf[b_][:], GT[:, j, t * 128:(t + 1) * 128], Yv[:, j, half * 512:(half + 1) * 512], start=(j == 0), stop=(j == jmax)),
                                     reads=[("GT", j), ("XY", j)], writes=[("psf", b_)])
                            hs = h[:, t, half * 512:(half + 1) * 512]
                            S.op("dve", lambda e, hs=hs, b_=b_: e.tensor_tensor(out=hs, in0=psf[b_][:], in1=hs, op=ALU.add), reads=[("psf", b_), ("h", t)], writes=[("h", t)])
                S.flush()
            S2 = Sched(nc, es, "g")
            cur[0] = S2
            with ExitStack() as st3:
                xnT = sb("xnTg", [128, KC, T], BF16, st3)
                bufs = swiglu_bufs(st3, GS=2)
                for t in range(NT):
                    pb = next_psb()
                    for k in range(KC):
                        S.op("pe", lambda e, k=k, t=t, pb=pb: e.transpose(psb[pb][:, k * 128:(k + 1) * 128], xn_tok[:, t, k * 128:(k + 1) * 128], ident_b[:]),
                             reads=[("xn_tok", t), "ident_b"], writes=[("psb", pb)])
                    S.op("act", lambda e, t=t, pb=pb: e.copy(out=xnT[:, :, t * 128:(t + 1) * 128], in_=psb[pb][:].rearrange("p (k c) -> p k c", k=KC)),
                         reads=[("psb", pb)], writes=[("xnT", t)])
                for ex in range(8):
                    swiglu_block(xnT, "xnT", moe_wg[ex], moe_wu[ex], moe_wd[ex], 3584, st3,
                                 scale_col=lambda t, ex=ex: (comb_ov[:, t, ex:ex + 1], "comb_ov"), bufs=bufs)
                S2.flush(guard=flag_d[0:1, 0:1], outer=S_main)
            cur[0] = S_main

    def phase_out(do_norm=True):
        with ExitStack() as st:
            es_attn.close()
            ob = [sb("ob%d" % i, [128, D], F32, st) for i in range(2)]
            gbc_ref[0] = sb("gbc_o", [128, D], F32, st)
            if do_norm:
                load_gain(final_norm)
                norm_stats()
            for t in range(NT):
                b = t % 2
                if do_norm:
                    S.op("dve", lambda e, t=t, b=b: e.scalar_tensor_tensor(out=ob[b][:], in0=h[:, t, :], scalar=rstd[:, t:t + 1], in1=gbc_ref[0][:], op0=ALU.mult, op1=ALU.mult),
                         reads=[("h", t), ("rstd", t), "gbc"], writes=[("ob", b)])
                else:
                    S.op("dve", lambda e, t=t, b=b: e.tensor_copy(out=ob[b][:], in_=h[:, t, :]), reads=[("h", t)], writes=[("ob", b)])
                dma_sp(out_d[t * 128:(t + 1) * 128, :], ob[b][:], ("out", b), reads=[("ob", b)])
            S.flush()
            S.final_wait()

    phases = [("mla", phase_mla), ("ffn", phase_ffn), ("dsw", phase_dsw), ("moe", phase_moe)]
    stopped = False
    for name, fn in phases:
        if stop == "none":
            stopped = True
            break
        fn()
        if stop.startswith(name):
            stopped = True
            break
    phase_out(do_norm=not stopped)
    es_attn.close()
    es.close()
    return nc


_CACHE = {}


def kernel(**inputs):
    stop = STOP
    if stop not in _CACHE:
        _CACHE[stop] = build_program(stop)
    nc = _CACHE[stop]
    consts = host_consts()
    x = np.asarray(inputs["x"], dtype=np.float32)
    pos = np.asarray(inputs["positions"], dtype=np.int32)
    shared = {}
    for k in ("norm_attn", "norm_ffn", "dsw_w_kv"):
        shared[k] = np.ascontiguousarray(np.asarray(inputs[k], dtype=np.float32))
    for k in ("mla_w_down", "mla_w_uq", "mla_w_ukv", "mla_w_o", "dsw_w_q", "dsw_w_o", "ffn_w_gate", "ffn_w_up", "ffn_w_down",
              "moe_router", "moe_w_gate", "moe_w_up", "moe_w_down"):
        shared[k] = np.ascontiguousarray(np.asarray(inputs[k], dtype=np.float32)[0])
    shared["mla_q_norm"] = np.ascontiguousarray(np.asarray(inputs["mla_q_norm"], dtype=np.float32).reshape(1, 384))
    shared["mla_kv_norm"] = np.ascontiguousarray(np.asarray(inputs["mla_kv_norm"], dtype=np.float32).reshape(1, 256))
    shared["dsw_kv_norm"] = np.ascontiguousarray(np.asarray(inputs["dsw_kv_norm"], dtype=np.float32).reshape(1, D))
    shared["final_norm"] = np.ascontiguousarray(np.asarray(inputs["final_norm"], dtype=np.float32).reshape(1, D))
    shared.update(consts)
    ncores = int(os.environ.get("MK_CORES", "8"))
    in_maps = []
    for c in range(ncores):
        m = dict(shared)
        m["x"] = np.ascontiguousarray(x[c])
        m["pos"] = np.ascontiguousarray(pos[c].reshape(NT, 128).T)
        in_maps.append(m)
    res = run_bass_kernel_spmd(nc, in_maps, core_ids=list(range(ncores)))
    out = np.stack([np.asarray(r["out"], dtype=np.float32).reshape(T, D) for r in res.results], axis=0)
    if ncores < 8:
        out = np.concatenate([out, np.zeros((8 - ncores, T, D), np.float32)], axis=0)
    return out
```

```python
import math
import os
from contextlib import ExitStack

import numpy as np
import concourse.bass as bass
import concourse.mybir as mybir
from concourse.bass_utils import run_bass_kernel_spmd

F32 = mybir.dt.float32
BF16 = mybir.dt.bfloat16
I32 = mybir.dt.int32
ALU = mybir.AluOpType
AF = mybir.ActivationFunctionType
AX = mybir.AxisListType

T = 2048
NT = 16
D = 1024
KC = 8
EPS = 1e-6
NEG = -30000.0
CAP = int(os.environ.get("MK_CAP", "640"))
NJ = CAP // 128
ENGS = ["pe", "act", "dve", "pool", "sp"]


class Op:
    __slots__ = ("eng", "fn", "reads", "writes", "dma_key", "deps", "signal", "sem", "val", "idx")


class Sched:
    def __init__(self, nc, es, tag=""):
        self.nc = nc
        self.es = es
        self.tag = tag
        self.sem = {e: es.enter_context(nc.semaphore("s_" + tag + e)) for e in ENGS}
        self.sigcount = {e: 0 for e in ENGS}
        self.waited = {e: {} for e in ENGS}
        self.dma_sem = {}
        self.dma_cnt = {}
        self.ops = []
        self.last_writer = {}
        self.readers = {}
        self.n_total = 0

    def op(self, eng, fn, reads=(), writes=(), dma_key=None):
        o = Op()
        o.eng, o.fn, o.reads, o.writes, o.dma_key = eng, fn, tuple(reads), tuple(writes), dma_key
        o.deps = set()
        o.signal = dma_key is not None
        o.sem = None
        o.val = None
        o.idx = len(self.ops)
        for k in o.reads:
            w = self.last_writer.get(k)
            if w is not None:
                o.deps.add(w)
            if isinstance(k, tuple) and k[0] in ("psf", "psb"):
                for r in self.readers.get(k, ()):
                    if r.eng != eng:
                        o.deps.add(r)
        for k in o.writes:
            w = self.last_writer.get(k)
            if w is not None:
                if dma_key is not None and w.dma_key == dma_key and w.eng == eng:
                    o.deps |= w.deps
                else:
                    o.deps.add(w)
            for r in self.readers.get(k, ()):
                o.deps.add(r)
        o.deps.discard(o)
        for k in o.reads:
            self.readers.setdefault(k, []).append(o)
        for k in o.writes:
            self.last_writer[k] = o
            self.readers[k] = []
        if dma_key is not None and dma_key not in self.dma_sem:
            self.dma_sem[dma_key] = self.es.enter_context(self.nc.semaphore("d_" + self.tag + str(len(self.dma_sem))))
            self.dma_cnt[dma_key] = 0
        self.ops.append(o)
        return o

    def barrier_waits(self, ename, eng):
        wd = self.waited[ename]
        pre_sig = getattr(self, "_pre_sig", None) or {e: 0 for e in ENGS}
        pre_dma = getattr(self, "_pre_dma", None) or {}
        for x in ENGS:
            if x != ename and pre_sig[x] > 0 and wd.get(self.sem[x].name, 0) < pre_sig[x]:
                eng.wait_ge(self.sem[x], pre_sig[x])
                wd[self.sem[x].name] = pre_sig[x]
        for k, v in pre_dma.items():
            if v > 0 and wd.get(self.dma_sem[k].name, 0) < v:
                eng.wait_ge(self.dma_sem[k], v)
                wd[self.dma_sem[k].name] = v

    def flush(self, guard=None, outer=None):
        ops = self.ops
        if not ops:
            return
        last = {}
        for o in ops:
            last[o.eng] = o
            for d in o.deps:
                if d.dma_key is None and not (d.eng == "pe" and o.eng == "pe" and o.dma_key is None):
                    d.signal = True
        for e, o in last.items():
            o.signal = True
        for o in ops:
            if o.dma_key is not None:
                self.dma_cnt[o.dma_key] += 16
                o.sem = self.dma_sem[o.dma_key]
                o.val = self.dma_cnt[o.dma_key]
            elif o.signal:
                self.sigcount[o.eng] += 1
                o.sem = self.sem[o.eng]
                o.val = self.sigcount[o.eng]
        pre_sig = dict(self._pre_sig) if hasattr(self, "_pre_sig") else {e: 0 for e in ENGS}
        pre_dma = dict(self._pre_dma) if hasattr(self, "_pre_dma") else {}
        by_eng = {e: [o for o in ops if o.eng == e] for e in ENGS}

        def emit(ename, eng):
            wd = self.waited[ename]

            def wait(sem, val):
                if val <= 0:
                    return
                if wd.get(sem.name, 0) < val:
                    eng.wait_ge(sem, val)
                    wd[sem.name] = val

            for x in ENGS:
                if x != ename:
                    wait(self.sem[x], pre_sig[x])
            for k, v in pre_dma.items():
                wait(self.dma_sem[k], v)
            for o in by_eng[ename]:
                for d in sorted(o.deps, key=lambda z: z.idx):
                    if d.dma_key is None and d.eng == "pe" and ename == "pe" and o.dma_key is None:
                        continue
                    wait(d.sem, d.val)
                ins = o.fn(eng)
                if o.signal:
                    ins.then_inc(o.sem, 16 if o.dma_key is not None else 1)

        def emit_guarded(ename, eng):
            outer.barrier_waits(ename, eng)
            reg = eng.alloc_register("flag_%s_%d" % (ename, id(self) % 100000))
            eng.reg_load(reg, guard)
            with eng.If(eng.snap(reg) > 0):
                emit(ename, eng)
                for x in ENGS:
                    if x != ename and self.sigcount[x] > 0:
                        eng.wait_ge(self.sem[x], self.sigcount[x])
                for k, v in self.dma_cnt.items():
                    if v > 0:
                        eng.wait_ge(self.dma_sem[k], v)

        with self.nc.Block() as block:
            if guard is not None:
                block.tensor(lambda eng: emit_guarded("pe", eng))
                block.scalar(lambda eng: emit_guarded("act", eng))
                block.vector(lambda eng: emit_guarded("dve", eng))
                block.gpsimd(lambda eng: emit_guarded("pool", eng))
                block.sync(lambda eng: emit_guarded("sp", eng))
            else:
                if by_eng["pe"]:
                    block.tensor(lambda eng: emit("pe", eng))
                if by_eng["act"]:
                    block.scalar(lambda eng: emit("act", eng))
                if by_eng["dve"]:
                    block.vector(lambda eng: emit("dve", eng))
                if by_eng["pool"]:
                    block.gpsimd(lambda eng: emit("pool", eng))
                if by_eng["sp"]:
                    block.sync(lambda eng: emit("sp", eng))
        self._pre_sig = dict(self.sigcount)
        self._pre_dma = dict(self.dma_cnt)
        self.n_total += len(ops)
        self.ops = []
        self.last_writer = {}
        self.readers = {}

    def final_wait(self):
        pre_sig = dict(self._pre_sig)
        pre_dma = dict(self._pre_dma)

        def emit(ename, eng):
            for x in ENGS:
                if x != ename and pre_sig[x] > 0:
                    eng.wait_ge(self.sem[x], pre_sig[x])
            for k, v in pre_dma.items():
                if v > 0:
                    eng.wait_ge(self.dma_sem[k], v)

        with self.nc.Block() as block:
            block.sync(lambda eng: emit("sp", eng))
            block.vector(lambda eng: emit("dve", eng))


def host_consts():
    p = np.arange(128)[:, None]
    j = np.arange(128)[None, :]
    dif = j - p

    def m(cond):
        a = np.where(cond, 0.0, NEG).astype(np.float32)
        return np.tile(a, (1, 4))

    masks = np.stack(
        [
            m(dif >= 0),
            m(dif <= 0),
            m((dif >= 0) & (dif % 4 == 0)),
            m(dif % 4 == 0),
            m((dif <= 0) & (dif % 4 == 0)),
            m((dif >= 0) & (dif % 16 == 0)),
            m(dif % 16 == 0),
        ],
        axis=1,
    )
    masks01 = (masks == 0.0).astype(np.float32)
    ident = np.eye(128, dtype=np.float32)
    theta = np.float32(500000.0)
    f_mla = np.power(theta, -np.arange(16, dtype=np.float32) * np.float32(2.0 / 32)).astype(np.float32)
    f_dsw = np.power(theta, -np.arange(8, dtype=np.float32) * np.float32(2.0 / 16)).astype(np.float32)
    freqs = np.concatenate([f_mla, f_dsw])[None, :].repeat(128, 0).astype(np.float32)
    ltri = (np.arange(128)[:, None] < np.arange(128)[None, :]).astype(np.float32)
    iota = np.arange(CAP, dtype=np.float32)[None, :].repeat(128, 0)
    return {"c_masks": np.ascontiguousarray(masks01), "c_ident": ident, "c_freqs": np.ascontiguousarray(freqs),
            "c_ltri": np.ascontiguousarray(ltri), "c_iota": np.ascontiguousarray(iota)}


STOP = os.environ.get("MK_STOP", "")


def build_program(stop=""):
    nc = bass.Bass("TRN2", target_bir_lowering=False)

    def din(name, shape, dt=F32):
        return nc.dram_tensor(name, list(shape), dt, kind="ExternalInput").ap()

    x_d = din("x", [T, D])
    pos_d = din("pos", [128, NT], I32)
    norm_attn = din("norm_attn", [2, D])
    norm_ffn = din("norm_ffn", [2, D])
    w_down = din("mla_w_down", [D, 672])
    q_norm = din("mla_q_norm", [1, 384])
    w_uq = din("mla_w_uq", [384, 1536])
    kv_norm = din("mla_kv_norm", [1, 256])
    w_ukv = din("mla_w_ukv", [256, 2048])
    w_o_a = din("mla_w_o", [D, D])
    dsw_kv_norm = din("dsw_kv_norm", [1, D])
    w_kv = din("dsw_w_kv", [D, 6144])
    w_q = din("dsw_w_q", [D, 3072])
    w_o_b = din("dsw_w_o", [D, D])
    ffn_wg = din("ffn_w_gate", [D, 2816])
    ffn_wu = din("ffn_w_up", [D, 2816])
    ffn_wd = din("ffn_w_down", [2816, D])
    router = din("moe_router", [D, 8])
    moe_wg = din("moe_w_gate", [8, D, 3584])
    moe_wu = din("moe_w_up", [8, D, 3584])
    moe_wd = din("moe_w_down", [8, 3584, D])
    final_norm = din("final_norm", [1, D])
    c_masks = din("c_masks", [128, 7, 512])
    c_ident = din("c_ident", [128, 128])
    c_freqs = din("c_freqs", [128, 24])
    c_ltri = din("c_ltri", [128, 128])
    c_iota = din("c_iota", [128, CAP])
    flag_d = nc.dram_tensor("moe_flag", [1, 8], I32, kind="Internal").ap()
    out_d = nc.dram_tensor("out", [T, D], F32, kind="ExternalOutput").ap()

    es = ExitStack()
    S_main = Sched(nc, es)
    cur = [S_main]

    class _Proxy:
        def op(self, *a, **k):
            return cur[0].op(*a, **k)

        def flush(self, *a, **k):
            return cur[0].flush(*a, **k)

        def final_wait(self):
            return cur[0].final_wait()

    S = _Proxy()

    uid = [0]

    def sb(name, shape, dt, stack=None):
        uid[0] += 1
        return (stack or es).enter_context(nc.sbuf_tensor("%s_%d" % (name, uid[0]), list(shape), dt))

    def psum(name, shape, dt, stack=None):
        return (stack or es).enter_context(nc.psum_tensor(name, list(shape), dt))

    h = sb("h", [128, NT, D], F32)
    ident_b = sb("ident_b", [128, 128], BF16)
    ident_f = sb("ident_f", [128, 128], F32)
    freqs = sb("freqs", [128, 24], F32)
    posf = sb("posf", [128, NT], F32)
    posi = sb("posi", [128, NT], I32)
    ss = sb("ss", [128, NT], F32)
    rstd = sb("rstd", [128, NT], F32)
    sqj = sb("sqj", [128, D], BF16)
    pibias = sb("pibias", [128, 1], F32)
    epsb = sb("epsb", [128, 1], F32)
    es_attn = ExitStack()
    masks = sb("masks", [128, 7, 512], BF16, es_attn)
    cs_mla = sb("cs_mla", [128, NT, 2, 16], F32, es_attn)
    cs_dsw = sb("cs_dsw", [128, NT, 2, 8], F32, es_attn)
    xnb = [sb("xnb%d" % i, [128, D], BF16, es_attn) for i in range(2)]
    gbc_ref = [sb("gbc", [128, D], F32, es_attn)]
    es_init = ExitStack()
    rq_i = sb("rq_i", [128, 256], I32, es_init)
    rq_f = sb("rq_f", [128, 256], F32, es_init)
    rq_m = sb("rq_m", [128, 256], F32, es_init)

    psf = [psum("psf%d" % i, [128, 512], F32) for i in range(6)]
    psb = [psum("psb%d" % i, [128, 1024], BF16) for i in range(2)]

    cnt = {"psf": 0, "psb": 0, "xnb": 0, "pso": 0}

    def next_psf():
        i = cnt["psf"] % 4
        cnt["psf"] += 1
        return i

    def next_pso():
        i = 4 + cnt["pso"] % 2
        cnt["pso"] += 1
        return i

    def next_psb():
        i = cnt["psb"] % 2
        cnt["psb"] += 1
        return i

    class Pipe:
        def __init__(self, L):
            self.L = L
            self.q = []

        def push(self, front, back):
            front()
            self.q.append(back)
            while len(self.q) > self.L:
                self.q.pop(0)()

        def drain(self):
            while self.q:
                self.q.pop(0)()

    def dma_sp(out, in_, key, reads=(), writes=(), slow=False):
        if slow:
            S.op("sp", lambda e: e.dma_start(out=out, in_=in_, allow_slow_non_contiguous=True), reads=reads, writes=writes, dma_key=key)
        else:
            S.op("sp", lambda e: e.dma_start(out=out, in_=in_), reads=reads, writes=writes, dma_key=key)

    def dma_cast(out, in_, key, reads=(), writes=()):
        S.op("pool", lambda e: e.dma_start(out=out, in_=in_), reads=reads, writes=writes, dma_key=key)

    def load_w(dst, src, key, nchunk, rows=128):
        v = src.rearrange("(c p) n -> p c n", p=rows)
        for c in range(nchunk):
            dma_cast(dst[:, c, :], v[:, c, :], key, writes=[key])

    def load_gain(src_row):
        dma_sp(gbc_ref[0][:], src_row.partition_broadcast(128), "gbc", writes=["gbc"])

    def norm_stats():
        for t in range(NT):
            S.op(
                "act",
                lambda e, t=t: e.activation(out=sqj[:], in_=h[:, t, :], func=AF.Square, accum_out=ss[:, t : t + 1]),
                reads=[("h", t)],
                writes=["sqj", ("ss", t)],
            )
        allss = [("ss", t) for t in range(NT)]
        allr = [("rstd", t) for t in range(NT)]
        S.op("act", lambda e: e.activation(out=rstd[:], in_=ss[:], func=AF.Sqrt, bias=epsb[:], scale=1.0 / D), reads=allss + ["epsb"], writes=allr)
        S.op("dve", lambda e: e.reciprocal(out=rstd[:], in_=rstd[:]), reads=allr, writes=allr)

    def norm_to_T(dstT, dkey, use_gain=True):
        for t in range(NT):
            b = cnt["xnb"] % 2
            cnt["xnb"] += 1
            if use_gain:
                S.op(
                    "dve",
                    lambda e, t=t, b=b: e.scalar_tensor_tensor(
                        out=xnb[b][:], in0=h[:, t, :], scalar=rstd[:, t : t + 1], in1=gbc_ref[0][:], op0=ALU.mult, op1=ALU.mult
                    ),
                    reads=[("h", t), ("rstd", t), "gbc"],
                    writes=[("xnb", b)],
                )
            else:
                S.op(
                    "dve",
                    lambda e, t=t, b=b: e.tensor_scalar(
                        out=xnb[b][:], in0=h[:, t, :], scalar1=rstd[:, t : t + 1], scalar2=None, op0=ALU.mult
                    ),
                    reads=[("h", t), ("rstd", t)],
                    writes=[("xnb", b)],
                )
            pb = next_psb()
            for k in range(KC):
                S.op(
                    "pe",
                    lambda e, k=k, b=b, pb=pb: e.transpose(psb[pb][:, k * 128 : (k + 1) * 128], xnb[b][:, k * 128 : (k + 1) * 128], ident_b[:]),
                    reads=[("xnb", b), "ident_b"],
                    writes=[("psb", pb)],
                )
            S.op(
                "act",
                lambda e, t=t, pb=pb: e.copy(out=dstT[:, :, t * 128 : (t + 1) * 128], in_=psb[pb][:].rearrange("p (k c) -> p k c", k=KC)),
                reads=[("psb", pb)],
                writes=[(dkey, t)],
            )

    def add_proj_to_h(srcT, skey, wt, wkey, nk, scale_col=None):
        for t in range(NT):
            for half in range(2):
                pi = next_psf()
                for k in range(nk):
                    S.op(
                        "pe",
                        lambda e, k=k, t=t, half=half, pi=pi: e.matmul(
                            psf[pi][:],
                            srcT[:, k, t * 128 : (t + 1) * 128],
                            wt[:, k, half * 512 : (half + 1) * 512],
                            start=(k == 0),
                            stop=(k == nk - 1),
                        ),
                        reads=[(skey, t), wkey],
                        writes=[("psf", pi)],
                    )
                hs = h[:, t, half * 512 : (half + 1) * 512]
                if scale_col is None:
                    S.op(
                        "dve",
                        lambda e, hs=hs, pi=pi: e.tensor_tensor(out=hs, in0=psf[pi][:], in1=hs, op=ALU.add),
                        reads=[("psf", pi), ("h", t)],
                        writes=[("h", t)],
                    )
                else:
                    sc, sckey = scale_col(t)
                    S.op(
                        "dve",
                        lambda e, hs=hs, pi=pi, sc=sc: e.scalar_tensor_tensor(
                            out=hs, in0=psf[pi][:], scalar=sc, in1=hs, op0=ALU.mult, op1=ALU.add
                        ),
                        reads=[("psf", pi), ("h", t), sckey],
                        writes=[("h", t)],
                    )

    def swiglu_bufs(st, GS=4):
        return dict(
            GS=GS,
            wg=[sb("wg%d" % i, [128, KC, GS * 128], BF16, st) for i in range(2)],
            wu=[sb("wu%d" % i, [128, KC, GS * 128], BF16, st) for i in range(2)],
            wd=[sb("wd%d" % i, [128, GS, D], BF16, st) for i in range(2)],
            hT=sb("hT", [128, GS, T], BF16, st),
            sil=[sb("sil%d" % i, [128, 512], F32, st) for i in range(2)],
            cnt=[0],
        )

    def swiglu_block(xT, xkey, wg_src, wu_src, wd_src, F, st, scale_col=None, bufs=None):
        if bufs is None:
            bufs = swiglu_bufs(st)
        GS = bufs["GS"]
        nF = F // 128
        groups = [(g0, min(GS, nF - g0)) for g0 in range(0, nF, GS)]
        wg, wu, wd, hT, sil = bufs["wg"], bufs["wu"], bufs["wd"], bufs["hT"], bufs["sil"]
        base = bufs["cnt"][0]
        bufs["cnt"][0] += len(groups)
        wgv = wg_src.rearrange("(c p) n -> p c n", p=128)
        wuv = wu_src.rearrange("(c p) n -> p c n", p=128)
        wdv = wd_src.rearrange("(c p) n -> p c n", p=128)

        def load_group(gi):
            g0, G = groups[gi]
            s = (base + gi) % 2
            for c in range(KC):
                dma_cast(wg[s][:, c, : G * 128], wgv[:, c, g0 * 128 : (g0 + G) * 128], ("wg", s), writes=[("wg", s)])
            for c in range(KC):
                dma_cast(wu[s][:, c, : G * 128], wuv[:, c, g0 * 128 : (g0 + G) * 128], ("wu", s), writes=[("wu", s)])
            for c in range(G):
                dma_cast(wd[s][:, c, :], wdv[:, g0 + c, :], ("wd", s), writes=[("wd", s)])

        load_group(0)
        nsil = 0
        for gi, (g0, G) in enumerate(groups):
            s = (base + gi) % 2
            if gi + 1 < len(groups):
                load_group(gi + 1)
            for fc in range(G):
                for nb in range(4):
                    pg = next_psf()
                    pu = next_psf()
                    for k in range(KC):
                        S.op(
                            "pe",
                            lambda e, k=k, fc=fc, nb=nb, pg=pg, s=s: e.matmul(
                                psf[pg][:], wg[s][:, k, fc * 128 : (fc + 1) * 128], xT[:, k, nb * 512 : (nb + 1) * 512],
                                start=(k == 0), stop=(k == KC - 1),
                            ),
                            reads=[("wg", s)] + [(xkey, nb * 4 + i) for i in range(4)],
                            writes=[("psf", pg)],
                        )
                    for k in range(KC):
                        S.op(
                            "pe",
                            lambda e, k=k, fc=fc, nb=nb, pu=pu, s=s: e.matmul(
                                psf[pu][:], wu[s][:, k, fc * 128 : (fc + 1) * 128], xT[:, k, nb * 512 : (nb + 1) * 512],
                                start=(k == 0), stop=(k == KC - 1),
                            ),
                            reads=[("wu", s)] + [(xkey, nb * 4 + i) for i in range(4)],
                            writes=[("psf", pu)],
                        )
                    si = nsil % 2
                    nsil += 1
                    S.op(
                        "act",
                        lambda e, pg=pg, si=si: e.activation(out=sil[si][:], in_=psf[pg][:], func=AF.Silu),
                        reads=[("psf", pg)],
                        writes=[("sil", si)],
                    )
                    S.op(
                        "dve",
                        lambda e, pu=pu, si=si, fc=fc, nb=nb: e.tensor_tensor(
                            out=hT[:, fc, nb * 512 : (nb + 1) * 512], in0=psf[pu][:], in1=sil[si][:], op=ALU.mult
                        ),
                        reads=[("psf", pu), ("sil", si)],
                        writes=[("hT", nb * 4 + i) for i in range(4)],
                    )
            add_proj_to_h(hT, "hT", wd[s], ("wd", s), G, scale_col=scale_col)

    for t in range(NT):
        dma_sp(h[:, t, :], x_d[t * 128 : (t + 1) * 128, :], ("h_in", t), writes=[("h", t)])
    dma_cast(ident_b[:], c_ident, "ident_b", writes=["ident_b"])
    dma_sp(ident_f[:], c_ident, "ident_f", writes=["ident_f"])
    dma_cast(masks[:], c_masks, "masks", writes=["masks"])
    dma_sp(freqs[:], c_freqs, "freqs", writes=["freqs"])
    dma_sp(posi[:], pos_d, "posi", writes=["posi"])
    S.op("dve", lambda e: e.tensor_copy(out=posf[:], in_=posi[:]), reads=["posi"], writes=["posf"])
    S.op("dve", lambda e: e.memset(pibias[:], math.pi), writes=["pibias"])
    S.op("dve", lambda e: e.memset(epsb[:], EPS), writes=["epsb"])


    def rope_table(cs, nf, f0, key, ntab=NT, pcol0=0):
        n = ntab * nf
        pb_ = posf[:, pcol0:pcol0 + ntab].unsqueeze(2).to_broadcast([128, ntab, nf])
        fb_ = freqs[:, f0:f0 + nf].unsqueeze(1).to_broadcast([128, ntab, nf])
        rf = rq_f[:, 0:n].rearrange("p (t f) -> p t f", f=nf)
        ri = rq_i[:, 0:n].rearrange("p (t f) -> p t f", f=nf)
        rm = rq_m[:, 0:n].rearrange("p (t f) -> p t f", f=nf)
        for which in range(2):
            dst = cs[:, 0:ntab, which, :]
            wk = [(key, "w", which)]
            S.op("dve", lambda e, dst=dst: e.tensor_tensor(out=dst, in0=pb_, in1=fb_, op=ALU.mult), reads=["freqs", "posf"], writes=wk)
            if which == 0:
                S.op("dve", lambda e, dst=dst: e.tensor_scalar(out=dst, in0=dst, scalar1=math.pi / 2, scalar2=None, op0=ALU.add), reads=wk, writes=wk)
            S.op("dve", lambda e, dst=dst: e.tensor_scalar(out=rf, in0=dst, scalar1=1.0 / (2 * math.pi), scalar2=None, op0=ALU.mult), reads=wk, writes=["rq_f"])
            S.op("dve", lambda e: e.tensor_copy(out=ri, in_=rf), reads=["rq_f"], writes=["rq_i"])
            S.op("dve", lambda e: e.tensor_copy(out=rf, in_=ri), reads=["rq_i"], writes=["rq_f"])
            S.op("dve", lambda e, dst=dst: e.scalar_tensor_tensor(out=dst, in0=rf, scalar=-2 * math.pi, in1=dst, op0=ALU.mult, op1=ALU.add), reads=["rq_f"] + wk, writes=wk)
            S.op("dve", lambda e, dst=dst: e.tensor_scalar(out=rm, in0=dst, scalar1=math.pi, scalar2=-2 * math.pi, op0=ALU.is_gt, op1=ALU.mult), reads=wk, writes=["rq_m"])
            S.op("dve", lambda e, dst=dst: e.tensor_tensor(out=dst, in0=dst, in1=rm, op=ALU.add), reads=["rq_m"] + wk, writes=wk)
            S.op("dve", lambda e, dst=dst: e.tensor_scalar(out=rm, in0=dst, scalar1=-math.pi, scalar2=2 * math.pi, op0=ALU.is_lt, op1=ALU.mult), reads=wk, writes=["rq_m"])
            S.op("dve", lambda e, dst=dst: e.tensor_tensor(out=dst, in0=dst, in1=rm, op=ALU.add), reads=["rq_m"] + wk, writes=wk)
            S.op("act", lambda e, dst=dst: e.activation(out=dst, in_=dst, func=AF.Sin), reads=wk, writes=wk + [(key, t, which) for t in range(ntab)])

    rope_table(cs_mla, 16, 0, "cs_mla")
    rope_table(cs_dsw, 8, 16, "cs_dsw")
    S.flush()
    es_init.close()

    def phase_mla():
        st = ExitStack()
        cqT = sb("cqT", [128, 3, T], BF16, st)
        ckvT = sb("ckvT", [128, 2, T], BF16, st)
        krb = sb("krb", [128, NT, 32], BF16, st)
        with ExitStack() as st1:
            xnT = sb("xnT", [128, KC, T], BF16, st1)
            wdn = sb("wdn", [128, KC, 672], BF16, st1)
            gq = sb("gq", [128, 384], F32, st1)
            gkv = sb("gkv", [128, 256], F32, st1)
            dn = [sb("dn%d" % i, [128, 672], F32, st1) for i in range(2)]
            cb = [sb("cb%d" % i, [128, 640], BF16, st1) for i in range(2)]
            s2 = sb("s2", [128, NT, 2], F32, st1)
            r2 = sb("r2", [128, NT, 2], F32, st1)
            rt = [sb("rt%d" % i, [128, 4, 16], F32, st1) for i in range(2)]
            load_w(wdn, w_down, "wdn", KC)
            load_gain(norm_attn[0:1, :])
            dma_sp(gq[:], q_norm.partition_broadcast(128), "gq", writes=["gq"])
            dma_sp(gkv[:], kv_norm.partition_broadcast(128), "gkv", writes=["gkv"])
            norm_stats()
            norm_to_T(xnT, "xnT")
            for t in range(NT):
                b = t % 2
                p0 = next_psf()
                p1 = next_psf()
                for k in range(KC):
                    S.op("pe", lambda e, k=k, t=t, p0=p0: e.matmul(psf[p0][:], xnT[:, k, t * 128:(t + 1) * 128], wdn[:, k, 0:512], start=(k == 0), stop=(k == KC - 1)),
                         reads=[("xnT", t), "wdn"], writes=[("psf", p0)])
                for k in range(KC):
                    S.op("pe", lambda e, k=k, t=t, p1=p1: e.matmul(psf[p1][:, 0:160], xnT[:, k, t * 128:(t + 1) * 128], wdn[:, k, 512:672], start=(k == 0), stop=(k == KC - 1)),
                         reads=[("xnT", t), "wdn"], writes=[("psf", p1)])
                S.op("act", lambda e, p0=p0, b=b: e.copy(out=dn[b][:, 0:512], in_=psf[p0][:]), reads=[("psf", p0)], writes=[("dn", b, 0)])
                S.op("act", lambda e, p1=p1, b=b: e.copy(out=dn[b][:, 512:672], in_=psf[p1][:, 0:160]), reads=[("psf", p1)], writes=[("dn", b, 1)])
                S.op("act", lambda e, b=b, t=t: e.activation(out=sqj[:, 0:384], in_=dn[b][:, 0:384], func=AF.Square, accum_out=s2[:, t, 0:1]),
                     reads=[("dn", b, 0)], writes=["sqj", ("s2", t, 0)])
                S.op("act", lambda e, b=b, t=t: e.activation(out=sqj[:, 384:640], in_=dn[b][:, 384:640], func=AF.Square, accum_out=s2[:, t, 1:2]),
                     reads=[("dn", b, 0), ("dn", b, 1)], writes=["sqj", ("s2", t, 1)])
                S.op("dve", lambda e, t=t: e.tensor_scalar(out=r2[:, t, 0:1], in0=s2[:, t, 0:1], scalar1=1.0 / 384, scalar2=EPS, op0=ALU.mult, op1=ALU.add),
                     reads=[("s2", t, 0)], writes=[("r2", t, 0)])
                S.op("dve", lambda e, t=t: e.tensor_scalar(out=r2[:, t, 1:2], in0=s2[:, t, 1:2], scalar1=1.0 / 256, scalar2=EPS, op0=ALU.mult, op1=ALU.add),
                     reads=[("s2", t, 1)], writes=[("r2", t, 1)])
                S.op("act", lambda e, t=t: e.activation(out=r2[:, t, :], in_=r2[:, t, :], func=AF.Sqrt),
                     reads=[("r2", t, 0), ("r2", t, 1)], writes=[("r2", t, 0), ("r2", t, 1)])
                S.op("dve", lambda e, t=t: e.reciprocal(out=r2[:, t, :], in_=r2[:, t, :]),
                     reads=[("r2", t, 0), ("r2", t, 1)], writes=[("r2", t, 0), ("r2", t, 1)])
                S.op("dve", lambda e, b=b, t=t: e.scalar_tensor_tensor(out=cb[b][:, 0:384], in0=dn[b][:, 0:384], scalar=r2[:, t, 0:1], in1=gq[:], op0=ALU.mult, op1=ALU.mult),
                     reads=[("dn", b, 0), ("r2", t, 0), "gq"], writes=[("cb", b)])
                S.op("dve", lambda e, b=b, t=t: e.scalar_tensor_tensor(out=cb[b][:, 384:640], in0=dn[b][:, 384:640], scalar=r2[:, t, 1:2], in1=gkv[:], op0=ALU.mult, op1=ALU.mult),
                     reads=[("dn", b, 0), ("dn", b, 1), ("r2", t, 1), "gkv"], writes=[("cb", b)])
                x1 = dn[b][:, 640:656]
                x2 = dn[b][:, 656:672]
                co = cs_mla[:, t, 0, :]
                si = cs_mla[:, t, 1, :]
                S.op("dve", lambda e, b=b, x1=x1, co=co: e.tensor_tensor(out=rt[b][:, 0, :], in0=x1, in1=co, op=ALU.mult), reads=[("dn", b, 1), ("cs_mla", t, 0)], writes=[("rt", b)])
                S.op("dve", lambda e, b=b, x2=x2, si=si: e.tensor_tensor(out=rt[b][:, 1, :], in0=x2, in1=si, op=ALU.mult), reads=[("dn", b, 1), ("cs_mla", t, 1)], writes=[("rt", b)])
                S.op("dve", lambda e, b=b, x2=x2, co=co: e.tensor_tensor(out=rt[b][:, 2, :], in0=x2, in1=co, op=ALU.mult), reads=[("dn", b, 1), ("cs_mla", t, 0)], writes=[("rt", b)])
                S.op("dve", lambda e, b=b, x1=x1, si=si: e.tensor_tensor(out=rt[b][:, 3, :], in0=x1, in1=si, op=ALU.mult), reads=[("dn", b, 1), ("cs_mla", t, 1)], writes=[("rt", b)])
                S.op("dve", lambda e, b=b, t=t: e.tensor_tensor(out=krb[:, t, 0:16], in0=rt[b][:, 0, :], in1=rt[b][:, 1, :], op=ALU.subtract), reads=[("rt", b)], writes=[("krb", t)])
                S.op("dve", lambda e, b=b, t=t: e.tensor_tensor(out=krb[:, t, 16:32], in0=rt[b][:, 2, :], in1=rt[b][:, 3, :], op=ALU.add), reads=[("rt", b)], writes=[("krb", t)])
                pb = next_psb()
                for k in range(5):
                    S.op("pe", lambda e, k=k, b=b, pb=pb: e.transpose(psb[pb][:, k * 128:(k + 1) * 128], cb[b][:, k * 128:(k + 1) * 128], ident_b[:]),
                         reads=[("cb", b), "ident_b"], writes=[("psb", pb)])
                S.op("act", lambda e, t=t, pb=pb: e.copy(out=cqT[:, :, t * 128:(t + 1) * 128], in_=psb[pb][:, 0:384].rearrange("p (k c) -> p k c", k=3)),
                     reads=[("psb", pb)], writes=[("cqT", t)])
                S.op("act", lambda e, t=t, pb=pb: e.copy(out=ckvT[:, :, t * 128:(t + 1) * 128], in_=psb[pb][:, 384:640].rearrange("p (k c) -> p k c", k=2)),
                     reads=[("psb", pb)], writes=[("ckvT", t)])
            S.flush()
        if stop == "mla_a":
            st.close()
            return
        with ExitStack() as st2:
            wuq = sb("wuq", [128, 3, 1536], BF16, st2)
            wukv = sb("wukv", [128, 2, 2048], BF16, st2)
            woa = sb("woa", [128, KC, D], BF16, st2)
            OT = sb("OT", [128, KC, T], BF16, st2)
            qtok = [sb("qtok%d" % i, [128, 2, 96], BF16, st2) for i in range(3)]
            ktok = [sb("ktok%d" % i, [128, 2, 96], BF16, st2) for i in range(3)]
            qrt = [sb("qrt%d" % i, [128, 4, 2, 16], F32, st2) for i in range(3)]
            QT = sb("QT", [96, 2, T], BF16, st2)
            KT = sb("KT", [96, 2, T], BF16, st2)
            VA = sb("VA", [128, NT, 2, 128], BF16, st2)
            PT = [sb("PT%d" % i, [128, 512], BF16, st2) for i in range(4)]
            rec = [sb("rec%d" % i, [64, 512], F32, st2) for i in range(2)]
            load_w(wuq, w_uq, "wuq", 3)
            load_w(wukv, w_ukv, "wukv", 2)
            load_w(woa, w_o_a, "woa", KC)
            S.op("dve", lambda e: e.memset(VA[:], 1.0), writes=["VA1"])
            npt = 0
            sc = 1.0 / math.sqrt(96.0)
            for hp in range(int(os.environ.get("MK_HP", "8"))):
                tpipe = Pipe(2)
                for t in range(NT):
                  def front_t(t=t, hp=hp):
                    b = t % 3
                    pq = next_psf()
                    for k in range(3):
                        S.op("pe", lambda e, k=k, t=t, pq=pq, hp=hp: e.matmul(psf[pq][:, 0:192], cqT[:, k, t * 128:(t + 1) * 128], wuq[:, k, hp * 192:(hp + 1) * 192], start=(k == 0), stop=(k == 2)),
                             reads=[("cqT", t), "wuq"], writes=[("psf", pq)])
                    pk = next_psf()
                    for k in range(2):
                        S.op("pe", lambda e, k=k, t=t, pk=pk, hp=hp: e.matmul(psf[pk][:, 0:256], ckvT[:, k, t * 128:(t + 1) * 128], wukv[:, k, hp * 256:(hp + 1) * 256], start=(k == 0), stop=(k == 1)),
                             reads=[("ckvT", t), "wukv"], writes=[("psf", pk)])
                    q3 = psf[pq][:, 0:192].rearrange("p (h d) -> p h d", h=2)
                    kv3 = psf[pk][:, 0:256].rearrange("p (h d) -> p h d", h=2)
                    S.op("act", lambda e, b=b, q3=q3: e.copy(out=qtok[b][:, :, 0:64], in_=q3[:, :, 0:64]), reads=[("psf", pq)], writes=[("qtok", b)])
                    co = cs_mla[:, t, 0:1, :].to_broadcast([128, 2, 16])
                    si = cs_mla[:, t, 1:2, :].to_broadcast([128, 2, 16])
                    x1 = q3[:, :, 64:80]
                    x2 = q3[:, :, 80:96]
                    S.op("dve", lambda e, b=b, x1=x1, co=co: e.tensor_tensor(out=qrt[b][:, 0], in0=x1, in1=co, op=ALU.mult), reads=[("psf", pq), ("cs_mla", t, 0)], writes=[("qrt", b)])
                    S.op("dve", lambda e, b=b, x2=x2, si=si: e.tensor_tensor(out=qrt[b][:, 1], in0=x2, in1=si, op=ALU.mult), reads=[("psf", pq), ("cs_mla", t, 1)], writes=[("qrt", b)])
                    S.op("dve", lambda e, b=b, x2=x2, co=co: e.tensor_tensor(out=qrt[b][:, 2], in0=x2, in1=co, op=ALU.mult), reads=[("psf", pq), ("cs_mla", t, 0)], writes=[("qrt", b)])
                    S.op("dve", lambda e, b=b, x1=x1, si=si: e.tensor_tensor(out=qrt[b][:, 3], in0=x1, in1=si, op=ALU.mult), reads=[("psf", pq), ("cs_mla", t, 1)], writes=[("qrt", b)])
                    S.op("dve", lambda e, b=b: e.tensor_tensor(out=qtok[b][:, :, 64:80], in0=qrt[b][:, 0], in1=qrt[b][:, 1], op=ALU.subtract), reads=[("qrt", b)], writes=[("qtok", b)])
                    S.op("dve", lambda e, b=b: e.tensor_tensor(out=qtok[b][:, :, 80:96], in0=qrt[b][:, 2], in1=qrt[b][:, 3], op=ALU.add), reads=[("qrt", b)], writes=[("qtok", b)])
                    S.op("act", lambda e, b=b, kv3=kv3: e.copy(out=ktok[b][:, :, 0:64], in_=kv3[:, :, 0:64]), reads=[("psf", pk)], writes=[("ktok", b)])
                    S.op("dve", lambda e, b=b, t=t: e.tensor_copy(out=ktok[b][:, :, 64:96], in_=krb[:, t:t + 1, :].to_broadcast([128, 2, 32])), reads=[("krb", t)], writes=[("ktok", b)])
                    S.op("dve", lambda e, t=t, kv3=kv3: e.tensor_copy(out=VA[:, t, :, 0:64], in_=kv3[:, :, 64:128]), reads=[("psf", pk), "VA1"], writes=[("VA", t)])
                  def back_t(t=t):
                    b = t % 3
                    pb = next_psb()
                    for hh in range(2):
                        S.op("pe", lambda e, hh=hh, b=b, pb=pb: e.transpose(psb[pb][0:96, hh * 128:(hh + 1) * 128], qtok[b][:, hh, :], ident_b[:]),
                             reads=[("qtok", b), "ident_b"], writes=[("psb", pb)])
                    for hh in range(2):
                        S.op("pe", lambda e, hh=hh, b=b, pb=pb: e.transpose(psb[pb][0:96, 256 + hh * 128:256 + (hh + 1) * 128], ktok[b][:, hh, :], ident_b[:]),
                             reads=[("ktok", b), "ident_b"], writes=[("psb", pb)])
                    S.op("act", lambda e, t=t, pb=pb: e.copy(out=QT[:, :, t * 128:(t + 1) * 128], in_=psb[pb][0:96, 0:256].rearrange("p (h c) -> p h c", h=2)),
                         reads=[("psb", pb)], writes=[("QT", t)])
                    S.op("act", lambda e, t=t, pb=pb: e.copy(out=KT[:, :, t * 128:(t + 1) * 128], in_=psb[pb][0:96, 256:512].rearrange("p (h c) -> p h c", h=2)),
                         reads=[("psb", pb)], writes=[("KT", t)])
                  tpipe.push(front_t, back_t)
                tpipe.drain()
                apipe = Pipe(3)
                for hh in range(2 if os.environ.get("MK_ATT", "1") == "1" else 0):
                    for qb in range(int(os.environ.get("MK_QB", "4"))):
                        po = next_pso()
                        nkt = 4 * qb + 4
                        for kt in range(nkt):
                            c0 = max(0, kt - 4 * qb) * 128
                            diag = kt >= 4 * qb
                            psi = next_psf()
                            pt = npt % 4
                            npt += 1

                            def front(hh=hh, qb=qb, kt=kt, c0=c0, psi=psi, diag=diag, pt=pt):
                                S.op("pe", lambda e: e.matmul(
                                    psf[psi][:, c0:512], KT[:, hh, kt * 128:(kt + 1) * 128], QT[:, hh, qb * 512 + c0:(qb + 1) * 512], start=True, stop=True),
                                    reads=[("KT", kt)] + [("QT", qb * 4 + i) for i in range(4)], writes=[("psf", psi)])
                                S.op("act", lambda e: e.activation(out=PT[pt][:, c0:512], in_=psf[psi][:, c0:512], func=AF.Exp, scale=sc),
                                     reads=[("psf", psi)], writes=[("PT", pt)])
                                if diag:
                                    S.op("dve", lambda e: e.tensor_tensor(out=PT[pt][:, c0:c0 + 128], in0=PT[pt][:, c0:c0 + 128], in1=masks[:, 0, 0:128], op=ALU.mult),
                                         reads=[("PT", pt), "masks"], writes=[("PT", pt)])

                            def back(hh=hh, qb=qb, kt=kt, c0=c0, po=po, pt=pt, nkt=nkt, hp=hp):
                                S.op("pe", lambda e: e.matmul(
                                    psf[po][:, c0:512], VA[:, kt, hh, :], PT[pt][:, c0:512], start=(kt == 0), stop=(kt == nkt - 1)),
                                    reads=[("VA", kt), "VA1", ("PT", pt)], writes=[("psf", po)])
                                if kt == nkt - 1:
                                    r = (hh + qb) % 2
                                    S.op("act", lambda e: e.activation(out=rec[r][:], in_=psf[po][64:128, :], func=AF.Ln), reads=[("psf", po)], writes=[("rec", r)])
                                    S.op("act", lambda e: e.activation(out=rec[r][:], in_=rec[r][:], func=AF.Exp, scale=-1.0), reads=[("rec", r)], writes=[("rec", r)])
                                    S.op("dve", lambda e: e.tensor_tensor(
                                        out=OT[hh * 64:(hh + 1) * 64, hp, qb * 512:(qb + 1) * 512], in0=psf[po][0:64, :], in1=rec[r][:], op=ALU.mult),
                                        reads=[("psf", po), ("rec", r)], writes=[("OT", qb * 4 + i) for i in range(4)])

                            apipe.push(front, back)
                apipe.drain()
            if os.environ.get("MK_WO", "1") == "1":
                add_proj_to_h(OT, "OT", woa, "woa", KC)
            S.flush()
        st.close()

    def phase_ffn():
        with ExitStack() as st:
            xnT = sb("xnT", [128, KC, T], BF16, st)
            load_gain(norm_ffn[0:1, :])
            norm_stats()
            norm_to_T(xnT, "xnT")
            swiglu_block(xnT, "xnT", ffn_wg, ffn_wu, ffn_wd, 2816, st)
            S.flush()

    def phase_dsw():
        with ExitStack() as st:
            hnT = sb("hnT", [128, KC, T], BF16, st)
            gq = sb("gq1", [128, KC], F32, st)
            gk = sb("gk1", [128, KC], F32, st)
            wq = [sb("wq%d" % i, [128, KC, 384], BF16, st) for i in range(1)]
            wk = [sb("wk%d" % i, [128, KC, 384], BF16, st) for i in range(1)]
            wv = [sb("wv%d" % i, [128, KC, 384], BF16, st) for i in range(1)]
            wo2 = [sb("wo2%d" % i, [128, 1, D], BF16, st) for i in range(2)]
            OTp = [sb("OTp%d" % i, [128, 1, T], BF16, st) for i in range(2)]
            qtok = [sb("qtk%d" % i, [128, 6, 64], BF16, st) for i in range(2)]
            ktok = [sb("ktk%d" % i, [128, 6, 64], BF16, st) for i in range(2)]
            rtq = [sb("rtq%d" % i, [128, 4, 6, 8], F32, st) for i in range(2)]
            rtk = [sb("rtk%d" % i, [128, 4, 6, 8], F32, st) for i in range(2)]
            QT = sb("QTb", [128, 3, T], BF16, st)
            KT = sb("KTb", [128, 3, T], BF16, st)
            VA = sb("VAb", [128, 3, NT, 2, 128], BF16, st)
            PT = [sb("PTb%d" % i, [128, 512], BF16, st) for i in range(4)]
            rec = [sb("recb%d" % i, [64, 512], F32, st) for i in range(1)]
            dma_sp(gq[:], norm_attn[1:2, :].rearrange("o (c p) -> p (o c)", p=128), "gq1", writes=["gq1"], slow=True)
            dma_sp(gk[:], dsw_kv_norm.rearrange("o (c p) -> p (o c)", p=128), "gk1", writes=["gk1"], slow=True)
            norm_stats()
            norm_to_T(hnT, "hnT", use_gain=False)
            S.op("dve", lambda e: e.memset(VA[:], 1.0), writes=["VA1"])
            wqv = w_q.rearrange("(c p) n -> p c n", p=128)
            wkvv = w_kv.rearrange("(c p) n -> p c n", p=128)

            def load_pair(hp):
                s = 0
                dma_cast(wo2[hp % 2][:, 0, :], w_o_b[hp * 128:(hp + 1) * 128, :], ("wo2", hp % 2), writes=[("wo2", hp % 2)])
                for g in range(3):
                    for c in range(KC):
                        dma_cast(wq[s][:, c, g * 128:(g + 1) * 128], wqv[:, c, g * 1024 + hp * 128: g * 1024 + (hp + 1) * 128], ("wq", s), writes=[("wq", s)])
                for g in range(3):
                    for c in range(KC):
                        dma_cast(wk[s][:, c, g * 128:(g + 1) * 128], wkvv[:, c, g * 1024 + hp * 128: g * 1024 + (hp + 1) * 128], ("wk", s), writes=[("wk", s)])
                for g in range(3):
                    for c in range(KC):
                        dma_cast(wv[s][:, c, g * 128:(g + 1) * 128], wkvv[:, c, 3072 + g * 1024 + hp * 128: 3072 + g * 1024 + (hp + 1) * 128], ("wv", s), writes=[("wv", s)])
                for c in range(KC):
                    S.op("dve", lambda e, s=s, c=c: e.tensor_scalar(out=wq[s][:, c, :], in0=wq[s][:, c, :], scalar1=gq[:, c:c + 1], scalar2=None, op0=ALU.mult),
                         reads=[("wq", s), "gq1"], writes=[("wq", s)])
                    S.op("dve", lambda e, s=s, c=c: e.tensor_scalar(out=wk[s][:, c, :], in0=wk[s][:, c, :], scalar1=gk[:, c:c + 1], scalar2=None, op0=ALU.mult),
                         reads=[("wk", s), "gk1"], writes=[("wk", s)])
                    S.op("dve", lambda e, s=s, c=c: e.tensor_scalar(out=wv[s][:, c, :], in0=wv[s][:, c, :], scalar1=gk[:, c:c + 1], scalar2=None, op0=ALU.mult),
                         reads=[("wv", s), "gk1"], writes=[("wv", s)])

            def rope6(ps3, dst, rt, b, t, pkey, dkey, rkey):
                co = cs_dsw[:, t, 0:1, :].to_broadcast([128, 6, 8])
                si = cs_dsw[:, t, 1:2, :].to_broadcast([128, 6, 8])
                x1 = ps3[:, :, 0:8]
                x2 = ps3[:, :, 8:16]
                S.op("act", lambda e: e.copy(out=dst[:, :, 16:64], in_=ps3[:, :, 16:64]), reads=[pkey], writes=[dkey])
                S.op("dve", lambda e: e.tensor_tensor(out=rt[:, 0], in0=x1, in1=co, op=ALU.mult), reads=[pkey, ("cs_dsw", t, 0)], writes=[rkey])
                S.op("dve", lambda e: e.tensor_tensor(out=rt[:, 1], in0=x2, in1=si, op=ALU.mult), reads=[pkey, ("cs_dsw", t, 1)], writes=[rkey])
                S.op("dve", lambda e: e.tensor_tensor(out=rt[:, 2], in0=x2, in1=co, op=ALU.mult), reads=[pkey, ("cs_dsw", t, 0)], writes=[rkey])
                S.op("dve", lambda e: e.tensor_tensor(out=rt[:, 3], in0=x1, in1=si, op=ALU.mult), reads=[pkey, ("cs_dsw", t, 1)], writes=[rkey])
                S.op("dve", lambda e: e.tensor_tensor(out=dst[:, :, 0:8], in0=rt[:, 0], in1=rt[:, 1], op=ALU.subtract), reads=[rkey], writes=[dkey])
                S.op("dve", lambda e: e.tensor_tensor(out=dst[:, :, 8:16], in0=rt[:, 2], in1=rt[:, 3], op=ALU.add), reads=[rkey], writes=[dkey])

            c6d = [0]

            def next6():
                i = c6d[0] % 6
                c6d[0] += 1
                return i

            load_pair(0)
            npt = 0
            sc = 1.0 / 8.0
            DIL = (1, 4, 16)
            MASK = {1: (0, None, 1), 4: (2, 3, 4), 16: (5, 6, None)}
            for hp in range(int(os.environ.get("MK_HP", "8"))):
                s = 0
                tpipe = Pipe(1)
                for t in range(NT):
                  def front_t(t=t, s=s):
                    b = t % 2
                    pq = next6()
                    pk = next6()
                    pv = next6()
                    for (pp, ww, wkey) in ((pq, wq, "wq"), (pk, wk, "wk"), (pv, wv, "wv")):
                        for k in range(KC):
                            S.op("pe", lambda e, k=k, t=t, pp=pp, ww=ww, s=s: e.matmul(psf[pp][:, 0:384], hnT[:, k, t * 128:(t + 1) * 128], ww[s][:, k, :], start=(k == 0), stop=(k == KC - 1)),
                                 reads=[("hnT", t), (wkey, s)], writes=[("psf", pp)])
                    q3 = psf[pq][:, 0:384].rearrange("p (s d) -> p s d", s=6)
                    k3 = psf[pk][:, 0:384].rearrange("p (s d) -> p s d", s=6)
                    rope6(q3, qtok[b], rtq[b], b, t, ("psf", pq), ("qtk", b), ("rtq", b))
                    rope6(k3, ktok[b], rtk[b], b, t, ("psf", pk), ("ktk", b), ("rtk", b))
                    S.op("act", lambda e, t=t, pv=pv: e.copy(out=VA[:, :, t, :, 0:64], in_=psf[pv][:, 0:384].rearrange("p (g h d) -> p g h d", g=3, h=2)),
                         reads=[("psf", pv), "VA1"], writes=[("VAb", t)])
                  def back_t(t=t):
                    b = t % 2
                    pb = next_psb()
                    for g in range(3):
                        S.op("pe", lambda e, g=g, b=b, pb=pb: e.transpose(psb[pb][:, g * 128:(g + 1) * 128], qtok[b][:, 2 * g:2 * g + 2, :].rearrange("p a d -> p (a d)"), ident_b[:]),
                             reads=[("qtk", b), "ident_b"], writes=[("psb", pb)])
                    for g in range(3):
                        S.op("pe", lambda e, g=g, b=b, pb=pb: e.transpose(psb[pb][:, 384 + g * 128:384 + (g + 1) * 128], ktok[b][:, 2 * g:2 * g + 2, :].rearrange("p a d -> p (a d)"), ident_b[:]),
                             reads=[("ktk", b), "ident_b"], writes=[("psb", pb)])
                    S.op("act", lambda e, t=t, pb=pb: e.copy(out=QT[:, :, t * 128:(t + 1) * 128], in_=psb[pb][:, 0:384].rearrange("p (g c) -> p g c", g=3)),
                         reads=[("psb", pb)], writes=[("QTb", t)])
                    S.op("act", lambda e, t=t, pb=pb: e.copy(out=KT[:, :, t * 128:(t + 1) * 128], in_=psb[pb][:, 384:768].rearrange("p (g c) -> p g c", g=3)),
                         reads=[("psb", pb)], writes=[("KTb", t)])
                  tpipe.push(front_t, back_t)
                tpipe.drain()
                if hp + 1 < 8:
                    load_pair(hp + 1)
                apipe = Pipe(3)
                for hh in range(2):
                    r0 = hh * 64
                    for qb in range(4):
                        po = next_pso()
                        contribs = []
                        for g in (2, 1, 0):
                            d = DIL[g]
                            for kt in range(0, 4 * qb + 4):
                                lo = max(kt, 4 * qb)
                                hi = min(kt + d, 4 * qb + 3)
                                if lo > hi:
                                    continue
                                contribs.append((g, kt, lo, hi))
                        assert contribs[0][2] == 4 * qb and contribs[0][3] == 4 * qb + 3
                        for ci, (g, kt, lo, hi) in enumerate(contribs):
                            d = DIL[g]
                            c0 = (lo - 4 * qb) * 128
                            c1 = (hi - 4 * qb + 1) * 128
                            psi = next_psf()
                            pt = npt % 4
                            npt += 1
                            segs = []
                            for qt in range(lo, hi + 1):
                                if qt == kt:
                                    mi = MASK[d][0]
                                elif qt - kt == d:
                                    mi = MASK[d][2]
                                else:
                                    mi = MASK[d][1]
                                a0 = (qt - 4 * qb) * 128
                                if mi is None:
                                    continue
                                if segs and segs[-1][0] == mi and mi == MASK[d][1] and segs[-1][2] == a0:
                                    segs[-1][2] = a0 + 128
                                else:
                                    segs.append([mi, a0, a0 + 128])

                            def front(g=g, kt=kt, c0=c0, c1=c1, psi=psi, r0=r0, qb=qb, segs=segs, pt=pt):
                                S.op("pe", lambda e: e.matmul(
                                    psf[psi][:, c0:c1], KT[r0:r0 + 64, g, kt * 128:(kt + 1) * 128], QT[r0:r0 + 64, g, qb * 512 + c0:qb * 512 + c1], start=True, stop=True),
                                    reads=[("KTb", kt)] + [("QTb", qb * 4 + i) for i in range(4)], writes=[("psf", psi)])
                                S.op("act", lambda e: e.activation(out=PT[pt][:, c0:c1], in_=psf[psi][:, c0:c1], func=AF.Exp, scale=sc),
                                     reads=[("psf", psi)], writes=[("PTb", pt)])
                                for si_, (mi, a0, a1) in enumerate(segs):
                                    S.op("dve", lambda e, mi=mi, a0=a0, a1=a1: e.tensor_tensor(out=PT[pt][:, a0:a1], in0=PT[pt][:, a0:a1], in1=masks[:, mi, 0:a1 - a0], op=ALU.mult),
                                         reads=[("PTb", pt), "masks"], writes=[("PTb", pt)])

                            def back(g=g, kt=kt, hh=hh, c0=c0, c1=c1, po=po, pt=pt, ci=ci, n=len(contribs), qb=qb, hp=hp):
                                S.op("pe", lambda e: e.matmul(
                                    psf[po][:, c0:c1], VA[:, g, kt, hh, :], PT[pt][:, c0:c1], start=(ci == 0), stop=(ci == n - 1)),
                                    reads=[("VAb", kt), "VA1", ("PTb", pt)], writes=[("psf", po)])
                                if ci == n - 1:
                                    r = 0
                                    S.op("act", lambda e: e.activation(out=rec[r][:], in_=psf[po][64:128, :], func=AF.Ln), reads=[("psf", po)], writes=[("recb", r)])
                                    S.op("act", lambda e: e.activation(out=rec[r][:], in_=rec[r][:], func=AF.Exp, scale=-1.0), reads=[("recb", r)], writes=[("recb", r)])
                                    S.op("dve", lambda e: e.tensor_tensor(
                                        out=OTp[hp % 2][hh * 64:(hh + 1) * 64, 0, qb * 512:(qb + 1) * 512], in0=psf[po][0:64, :], in1=rec[r][:], op=ALU.mult),
                                        reads=[("psf", po), ("recb", r)], writes=[(("OTp", hp % 2), qb * 4 + i) for i in range(4)])

                            apipe.push(front, back)
                apipe.drain()
                add_proj_to_h(OTp[hp % 2], ("OTp", hp % 2), wo2[hp % 2], ("wo2", hp % 2), 1)
            S.flush()

    def phase_moe_dense():
        with ExitStack() as st:
            xnT = sb("xnT", [128, KC, T], BF16, st)
            comb = sb("comb", [128, NT, 8], F32, st)
            rtr = sb("rtr", [128, KC, 8], F32, st)
            xnf = [sb("xnf%d" % i, [128, D], F32, st) for i in range(2)]
            xTf = [sb("xTf%d" % i, [128, D], F32, st) for i in range(2)]
            lg = sb("lg", [128, NT, 8], F32, st)
            m1 = sb("m1", [128, NT, 4], F32, st)
            tmp8 = sb("tmp8", [128, NT, 3, 8], F32, st)
            load_gain(norm_ffn[1:2, :])
            dma_sp(rtr[:], router.rearrange("(c p) e -> p c e", p=128), "rtr", writes=["rtr"])
            norm_stats()
            for t in range(NT):
                b = t % 2
                S.op("dve", lambda e, t=t, b=b: e.scalar_tensor_tensor(out=xnf[b][:], in0=h[:, t, :], scalar=rstd[:, t:t + 1], in1=gbc_ref[0][:], op0=ALU.mult, op1=ALU.mult),
                     reads=[("h", t), ("rstd", t), "gbc"], writes=[("xnf", b)])
                pa = next_psf()
                pb2 = next_psf()
                for k in range(KC):
                    pp = pa if k < 4 else pb2
                    S.op("pe", lambda e, k=k, b=b, pp=pp: e.transpose(psf[pp][:, (k % 4) * 128:(k % 4 + 1) * 128], xnf[b][:, k * 128:(k + 1) * 128], ident_f[:]),
                         reads=[("xnf", b), "ident_f"], writes=[("psf", pp)])
                S.op("act", lambda e, b=b, pa=pa: e.copy(out=xTf[b][:, 0:512], in_=psf[pa][:]), reads=[("psf", pa)], writes=[("xTf", b, 0)])
                S.op("act", lambda e, b=b, pb2=pb2: e.copy(out=xTf[b][:, 512:1024], in_=psf[pb2][:]), reads=[("psf", pb2)], writes=[("xTf", b, 1)])
                pl = next_psf()
                for k in range(KC):
                    S.op("pe", lambda e, k=k, b=b, pl=pl: e.matmul(psf[pl][:, 0:8], xTf[b][:, k * 128:(k + 1) * 128], rtr[:, k, :], start=(k == 0), stop=(k == KC - 1)),
                         reads=[("xTf", b, 0), ("xTf", b, 1), "rtr"], writes=[("psf", pl)])
                S.op("dve", lambda e, t=t, pl=pl: e.tensor_copy(out=lg[:, t, :], in_=psf[pl][:, 0:8]), reads=[("psf", pl)], writes=[("lg", t)])
                L = lg[:, t, :]
                S.op("dve", lambda e, t=t, L=L: e.reduce_max(out=m1[:, t, 0:1], in_=L, axis=AX.X), reads=[("lg", t)], writes=[("m1", t)])
                S.op("dve", lambda e, t=t, L=L: e.tensor_scalar(out=tmp8[:, t, 0, :], in0=L, scalar1=m1[:, t, 0:1], scalar2=-1e30, op0=ALU.is_ge, op1=ALU.mult),
                     reads=[("lg", t), ("m1", t)], writes=[("tmp8", t)])
                S.op("dve", lambda e, t=t, L=L: e.tensor_tensor(out=tmp8[:, t, 0, :], in0=tmp8[:, t, 0, :], in1=L, op=ALU.add), reads=[("tmp8", t), ("lg", t)], writes=[("tmp8", t)])
                S.op("dve", lambda e, t=t: e.reduce_max(out=m1[:, t, 1:2], in_=tmp8[:, t, 0, :], axis=AX.X), reads=[("tmp8", t)], writes=[("m1", t)])
                S.op("dve", lambda e, t=t, L=L: e.tensor_scalar(out=tmp8[:, t, 1, :], in0=L, scalar1=m1[:, t, 1:2], scalar2=None, op0=ALU.is_ge),
                     reads=[("lg", t), ("m1", t)], writes=[("tmp8", t)])
                S.op("dve", lambda e, t=t, L=L: e.tensor_scalar(out=tmp8[:, t, 2, :], in0=L, scalar1=m1[:, t, 0:1], scalar2=None, op0=ALU.subtract),
                     reads=[("lg", t), ("m1", t)], writes=[("tmp8", t)])
                S.op("act", lambda e, t=t: e.activation(out=tmp8[:, t, 2, :], in_=tmp8[:, t, 2, :], func=AF.Exp), reads=[("tmp8", t)], writes=[("tmp8", t)])
                S.op("dve", lambda e, t=t: e.tensor_tensor(out=tmp8[:, t, 2, :], in0=tmp8[:, t, 2, :], in1=tmp8[:, t, 1, :], op=ALU.mult), reads=[("tmp8", t)], writes=[("tmp8", t)])
                S.op("dve", lambda e, t=t: e.reduce_sum(out=m1[:, t, 2:3], in_=tmp8[:, t, 2, :], axis=AX.X), reads=[("tmp8", t)], writes=[("m1", t)])
                S.op("dve", lambda e, t=t: e.reciprocal(out=m1[:, t, 3:4], in_=m1[:, t, 2:3]), reads=[("m1", t)], writes=[("m1", t)])
                S.op("dve", lambda e, t=t: e.tensor_scalar(out=comb[:, t, :], in0=tmp8[:, t, 2, :], scalar1=m1[:, t, 3:4], scalar2=None, op0=ALU.mult),
                     reads=[("tmp8", t), ("m1", t)], writes=[("comb", t)])
            norm_to_T(xnT, "xnT")
            S.flush()
            for ex in range(8):
                with ExitStack() as st2:
                    swiglu_block(xnT, "xnT", moe_wg[ex], moe_wu[ex], moe_wd[ex], 3584, st2,
                                 scale_col=lambda t, ex=ex: (comb[:, t, ex:ex + 1], ("comb", t)))
                    S.flush()

    def phase_moe():
        es_attn.close()
        c6 = [0]

        def next6():
            i = c6[0] % 6
            c6[0] += 1
            return i

        with ExitStack() as st:
            xn_tok = sb("xn_tok", [128, NT, D], BF16, st)
            comb = sb("comb", [128, NT, 8], F32, st)
            comb_hl = sb("comb_hl", [128, NT, 8, 2], BF16, st)
            comb_ov = sb("comb_ov", [128, NT, 8], F32, st)
            pos = sb("pos", [128, NT, 8], F32, st)
            iota = sb("iota", [128, CAP], F32, st)
            with ExitStack() as st1:
                gbc_ref[0] = sb("gbc_r", [128, D], F32, st1)
                rtr = sb("rtr", [128, KC, 8], F32, st1)
                xnf = [sb("xnf%d" % i, [128, D], F32, st1) for i in range(2)]
                xTf = [sb("xTf%d" % i, [128, D], F32, st1) for i in range(2)]
                lg = sb("lg", [128, NT, 8], F32, st1)
                m1 = sb("m1", [128, NT, 4], F32, st1)
                tmp8 = sb("tmp8", [128, NT, 3, 8], F32, st1)
                selb = sb("selb", [128, NT, 8], BF16, st1)
                ltri = sb("ltri", [128, 128], BF16, st1)
                ones_b = sb("ones_b", [128, 128], BF16, st1)
                tot = sb("tot", [128, NT, 8], F32, st1)
                cum = sb("cum", [128, NT, 8], F32, st1)
                chi = sb("chi", [128, NT, 8], F32, st1)
                ne = sb("ne", [128, 8], F32, st1)
                flag_f = sb("flag_f", [128, 8], F32, st1)
                flag_i = sb("flag_i", [128, 8], I32, st1)
                load_gain(norm_ffn[1:2, :])
                dma_sp(rtr[:], router.rearrange("(c p) e -> p c e", p=128), "rtr", writes=["rtr"])
                dma_sp(iota[:], c_iota, "iota", writes=["iota"])
                dma_cast(ltri[:], c_ltri, "ltri", writes=["ltri"])
                S.op("dve", lambda e: e.memset(ones_b[:], 1.0), writes=["ones_b"])
                norm_stats()
                for t in range(NT):
                    b = t % 2
                    S.op("dve", lambda e, t=t, b=b: e.scalar_tensor_tensor(out=xnf[b][:], in0=h[:, t, :], scalar=rstd[:, t:t + 1], in1=gbc_ref[0][:], op0=ALU.mult, op1=ALU.mult),
                         reads=[("h", t), ("rstd", t), "gbc"], writes=[("xnf", b)])
                    S.op("act", lambda e, t=t, b=b: e.copy(out=xn_tok[:, t, :], in_=xnf[b][:]), reads=[("xnf", b)], writes=[("xn_tok", t)])
                    pa = next_psf()
                    pb2 = next_psf()
                    for k in range(KC):
                        pp = pa if k < 4 else pb2
                        S.op("pe", lambda e, k=k, b=b, pp=pp: e.transpose(psf[pp][:, (k % 4) * 128:(k % 4 + 1) * 128], xnf[b][:, k * 128:(k + 1) * 128], ident_f[:]),
                             reads=[("xnf", b), "ident_f"], writes=[("psf", pp)])
                    S.op("act", lambda e, b=b, pa=pa: e.copy(out=xTf[b][:, 0:512], in_=psf[pa][:]), reads=[("psf", pa)], writes=[("xTf", b, 0)])
                    S.op("act", lambda e, b=b, pb2=pb2: e.copy(out=xTf[b][:, 512:1024], in_=psf[pb2][:]), reads=[("psf", pb2)], writes=[("xTf", b, 1)])
                    pl = next_psf()
                    for k in range(KC):
                        S.op("pe", lambda e, k=k, b=b, pl=pl: e.matmul(psf[pl][:, 0:8], xTf[b][:, k * 128:(k + 1) * 128], rtr[:, k, :], start=(k == 0), stop=(k == KC - 1)),
                             reads=[("xTf", b, 0), ("xTf", b, 1), "rtr"], writes=[("psf", pl)])
                    S.op("dve", lambda e, t=t, pl=pl: e.tensor_copy(out=lg[:, t, :], in_=psf[pl][:, 0:8]), reads=[("psf", pl)], writes=[("lg", t)])
                allt = lambda nm: [(nm, t) for t in range(NT)]
                bc = lambda col: m1[:, :, col:col + 1].to_broadcast([128, NT, 8])
                T0, T1, T2 = tmp8[:, :, 0, :], tmp8[:, :, 1, :], tmp8[:, :, 2, :]
                S.op("dve", lambda e: e.reduce_max(out=m1[:, :, 0], in_=lg[:], axis=AX.X), reads=allt("lg"), writes=["m1a"])
                S.op("dve", lambda e: e.tensor_tensor(out=T0, in0=lg[:], in1=bc(0), op=ALU.is_ge), reads=allt("lg") + ["m1a"], writes=["T0"])
                S.op("dve", lambda e: e.scalar_tensor_tensor(out=T0, in0=T0, scalar=-1e30, in1=lg[:], op0=ALU.mult, op1=ALU.add), reads=allt("lg") + ["T0"], writes=["T0"])
                S.op("dve", lambda e: e.reduce_max(out=m1[:, :, 1], in_=T0, axis=AX.X), reads=["T0"], writes=["m1b"])
                S.op("dve", lambda e: e.tensor_tensor(out=T1, in0=lg[:], in1=bc(1), op=ALU.is_ge), reads=allt("lg") + ["m1b"], writes=allt("tmp8"))
                S.op("dve", lambda e: e.tensor_tensor(out=T2, in0=lg[:], in1=bc(0), op=ALU.subtract), reads=allt("lg") + ["m1a"], writes=["T2"])
                S.op("act", lambda e: e.activation(out=T2, in_=T2, func=AF.Exp), reads=["T2"], writes=["T2"])
                S.op("dve", lambda e: e.tensor_tensor(out=T2, in0=T2, in1=T1, op=ALU.mult), reads=["T2"] + allt("tmp8"), writes=["T2"])
                S.op("dve", lambda e: e.reduce_sum(out=m1[:, :, 2], in_=T2, axis=AX.X), reads=["T2"], writes=["m1c"])
                S.op("dve", lambda e: e.reciprocal(out=m1[:, :, 3], in_=m1[:, :, 2]), reads=["m1c"], writes=["m1d"])
                S.op("dve", lambda e: e.tensor_tensor(out=comb[:], in0=T2, in1=bc(3), op=ALU.mult), reads=["T2", "m1d"], writes=allt("comb"))
                S.op("dve", lambda e: e.tensor_copy(out=selb[:], in_=T1), reads=allt("tmp8"), writes=allt("selb"))
                S.op("pe", lambda e: e.matmul(psf[0][:, 0:128], ltri[:], selb[:].rearrange("p t e -> p (t e)"), start=True, stop=True), reads=["ltri"] + allt("selb"), writes=[("psf", 0)])
                S.op("pe", lambda e: e.matmul(psf[1][:, 0:128], ones_b[:], selb[:].rearrange("p t e -> p (t e)"), start=True, stop=True), reads=["ones_b"] + allt("selb"), writes=[("psf", 1)])
                S.op("dve", lambda e: e.tensor_copy(out=tot[:].rearrange("p t e -> p (t e)"), in_=psf[1][:, 0:128]), reads=[("psf", 1)], writes=["tot"])
                S.op("dve", lambda e: e.memset(cum[:, 0, :], 0.0), writes=[("cum", 0)])
                for t in range(1, NT):
                    S.op("dve", lambda e, t=t: e.tensor_tensor(out=cum[:, t, :], in0=cum[:, t - 1, :], in1=tot[:, t - 1, :], op=ALU.add), reads=[("cum", t - 1), "tot"], writes=[("cum", t)])
                S.op("dve", lambda e: e.tensor_tensor(out=pos[:].rearrange("p t e -> p (t e)"), in0=psf[0][:, 0:128], in1=cum[:].rearrange("p t e -> p (t e)"), op=ALU.add),
                     reads=[("psf", 0)] + allt("cum"), writes=["pos"])
                S.op("dve", lambda e: e.scalar_tensor_tensor(out=pos[:], in0=pos[:], scalar=1.0, in1=tmp8[:, :, 1, :], op0=ALU.add, op1=ALU.mult), reads=["pos"] + allt("tmp8"), writes=["pos"])
                S.op("dve", lambda e: e.tensor_scalar(out=pos[:], in0=pos[:], scalar1=-1.0, scalar2=None, op0=ALU.add), reads=["pos"], writes=["pos"])
                S.op("dve", lambda e: e.scalar_tensor_tensor(out=comb_ov[:], in0=pos[:], scalar=float(CAP), in1=comb[:], op0=ALU.is_ge, op1=ALU.mult), reads=["pos"] + allt("comb"), writes=["comb_ov"])
                S.op("dve", lambda e: e.tensor_tensor(out=ne[:], in0=cum[:, NT - 1, :], in1=tot[:, NT - 1, :], op=ALU.add), reads=[("cum", NT - 1), "tot"], writes=["ne"])
                S.op("dve", lambda e: e.reduce_max(out=flag_f[:, 0:1], in_=ne[:], axis=AX.X), reads=["ne"], writes=["flag_f0"])
                S.op("dve", lambda e: e.tensor_scalar(out=flag_f[:, 1:2], in0=flag_f[:, 0:1], scalar1=float(CAP), scalar2=None, op0=ALU.is_gt), reads=["flag_f0"], writes=["flag_f1"])
                S.op("dve", lambda e: e.tensor_copy(out=flag_i[:, 0:8], in_=flag_f[:, 1:2].to_broadcast([128, 8])), reads=["flag_f1"], writes=["flag_i"])
                dma_sp(flag_d, flag_i[0:1, :], "flag", reads=["flag_i"])
                S.op("dve", lambda e: e.tensor_copy(out=comb_hl[:, :, :, 0], in_=comb[:]), reads=allt("comb"), writes=["comb_h"])
                S.op("dve", lambda e: e.tensor_copy(out=chi[:], in_=comb_hl[:, :, :, 0]), reads=["comb_h"], writes=["chi"])
                S.op("dve", lambda e: e.tensor_tensor(out=comb_hl[:, :, :, 1], in0=comb[:], in1=chi[:], op=ALU.subtract), reads=["chi"] + allt("comb"), writes=["comb_l"])
                S.flush()
            with ExitStack() as st2:
                Sel = [sb("Sel%d" % i, [128, NT, 128], BF16, st2) for i in range(2)]
                GT = sb("GT", [128, NJ, T], BF16, st2)
                XY = sb("XY", [128, 8 * CAP], BF16, st2)
                XeT = XY[:, :].rearrange("p (k c) -> p k c", k=8)
                Yv = XY[:, :].rearrange("p (j d) -> p j d", j=NJ)
                hT = sb("hTs", [128, 28, CAP], BF16, st2)
                wg2 = [sb("wg2%d" % i, [128, KC, 256], BF16, st2) for i in range(3)]
                wu2 = [sb("wu2%d" % i, [128, KC, 256], BF16, st2) for i in range(3)]
                wd2 = [sb("wd2%d" % i, [128, 2, 512], BF16, st2) for i in range(2)]
                sil = [sb("sils%d" % i, [128, CAP // 2], F32, st2) for i in range(2)]
                gsl = sb("gsl", [128, 2 * NJ], F32, st2)
                gate_e = sb("gate_e", [128, NJ], F32, st2)
                HW_ = CAP // 2
                n_gu = 8 * 14
                n_d = 8 * 2 * 14

                def issue_gu(i):
                    ex, grp = divmod(i, 14)
                    s_ = i % 3
                    wgv = moe_wg[ex].rearrange("(c p) n -> p c n", p=128)
                    wuv = moe_wu[ex].rearrange("(c p) n -> p c n", p=128)
                    for c in range(KC):
                        dma_cast(wg2[s_][:, c, :], wgv[:, c, grp * 256:(grp + 1) * 256], ("wg2", s_), writes=[("wg2", s_)])
                    for c in range(KC):
                        dma_cast(wu2[s_][:, c, :], wuv[:, c, grp * 256:(grp + 1) * 256], ("wu2", s_), writes=[("wu2", s_)])

                def issue_d(i):
                    ex, r = divmod(i, 28)
                    oh, g2 = divmod(r, 14)
                    s_ = i % 2
                    wdv = moe_wd[ex].rearrange("(c p) n -> p c n", p=128)
                    for c in range(2):
                        dma_cast(wd2[s_][:, c, :], wdv[:, g2 * 2 + c, oh * 512:(oh + 1) * 512], ("wd2", s_), writes=[("wd2", s_)])

                issue_gu(0)
                issue_gu(1)
                issue_d(0)
                nsil = 0
                allXY = [("XY", j) for j in range(NJ)]
                for ex in range(8):
                    for j in range(NJ):
                        sl = j % 2
                        for t in range(j, NT):
                            S.op("dve", lambda e, j=j, t=t, sl=sl, ex=ex: e.tensor_scalar(out=Sel[sl][:, t, :], in0=iota[:, j * 128:(j + 1) * 128], scalar1=pos[:, t, ex:ex + 1], scalar2=None, op0=ALU.is_equal),
                                 reads=["pos", "iota"], writes=[("Sel", sl, t)])
                        ba, bb = (0, 1) if j % 2 == 0 else (2, 3)
                        for k in range(KC):
                            bank = ba if k < 4 else bb
                            for t in range(j, NT):
                                S.op("pe", lambda e, k=k, t=t, sl=sl, bank=bank, j=j: e.matmul(psf[bank][:, (k % 4) * 128:(k % 4 + 1) * 128], xn_tok[:, t, k * 128:(k + 1) * 128], Sel[sl][:, t, :], start=(t == j), stop=(t == NT - 1)),
                                     reads=[("xn_tok", t), ("Sel", sl, t)], writes=[("psf", bank)])
                        S.op("act", lambda e, j=j, ba=ba: e.copy(out=XeT[:, 0:4, j * 128:(j + 1) * 128], in_=psf[ba][:].rearrange("p (k c) -> p k c", k=4)), reads=[("psf", ba)], writes=[("XY", j)])
                        S.op("act", lambda e, j=j, bb=bb: e.copy(out=XeT[:, 4:8, j * 128:(j + 1) * 128], in_=psf[bb][:].rearrange("p (k c) -> p k c", k=4)), reads=[("psf", bb)], writes=[("XY", j)])
                        for t in range(j, NT):
                            S.op("pe", lambda e, j=j, t=t, sl=sl, ex=ex: e.matmul(psf[4][:, 2 * j:2 * j + 2], Sel[sl][:, t, :], comb_hl[:, t, ex, :], start=(t == j), stop=(t == NT - 1)),
                                 reads=[("Sel", sl, t), "comb_h", "comb_l"], writes=[("psf", 4)])
                        for t in range(j, NT):
                            pb = 0 if t < 8 else 1
                            S.op("pe", lambda e, t=t, sl=sl, pb=pb: e.transpose(psb[pb][:, (t % 8) * 128:(t % 8 + 1) * 128], Sel[sl][:, t, :], ident_b[:]),
                                 reads=[("Sel", sl, t), "ident_b"], writes=[("psb", pb)])
                        S.op("act", lambda e, j=j: e.copy(out=GT[:, j, j * 128:1024], in_=psb[0][:, j * 128:1024]), reads=[("psb", 0)], writes=[("GT", j)])
                        S.op("act", lambda e, j=j: e.copy(out=GT[:, j, 1024:2048], in_=psb[1][:]), reads=[("psb", 1)], writes=[("GT", j)])
                    S.op("dve", lambda e: e.tensor_copy(out=gsl[:], in_=psf[4][:, 0:2 * NJ]), reads=[("psf", 4)], writes=["gsl"])
                    g2v = gsl[:, :].rearrange("p (j two) -> p j two", two=2)
                    S.op("dve", lambda e, g2v=g2v: e.tensor_tensor(out=gate_e[:], in0=g2v[:, :, 0], in1=g2v[:, :, 1], op=ALU.add), reads=["gsl"], writes=["gate_e"])
                    for grp in range(14):
                        i = ex * 14 + grp
                        s_ = i % 3
                        if i + 2 < n_gu:
                            issue_gu(i + 2)
                        for fc in range(2):
                            c = grp * 2 + fc
                            for half in range(2):
                                pg = next6()
                                pu = next6()
                                for k in range(KC):
                                    S.op("pe", lambda e, k=k, fc=fc, half=half, pg=pg, s_=s_: e.matmul(psf[pg][:, 0:HW_], wg2[s_][:, k, fc * 128:(fc + 1) * 128], XeT[:, k, half * HW_:(half + 1) * HW_], start=(k == 0), stop=(k == KC - 1)),
                                         reads=[("wg2", s_)] + allXY, writes=[("psf", pg)])
                                for k in range(KC):
                                    S.op("pe", lambda e, k=k, fc=fc, half=half, pu=pu, s_=s_: e.matmul(psf[pu][:, 0:HW_], wu2[s_][:, k, fc * 128:(fc + 1) * 128], XeT[:, k, half * HW_:(half + 1) * HW_], start=(k == 0), stop=(k == KC - 1)),
                                         reads=[("wu2", s_)] + allXY, writes=[("psf", pu)])
                                si = nsil % 2
                                nsil += 1
                                S.op("act", lambda e, pg=pg, si=si: e.activation(out=sil[si][:], in_=psf[pg][:, 0:HW_], func=AF.Silu), reads=[("psf", pg)], writes=[("sils", si)])
                                S.op("dve", lambda e, pu=pu, si=si, c=c, half=half: e.tensor_tensor(out=hT[:, c, half * HW_:(half + 1) * HW_], in0=psf[pu][:, 0:HW_], in1=sil[si][:], op=ALU.mult),
                                     reads=[("psf", pu), ("sils", si)], writes=[("hTs", c)])
                    for oh in range(2):
                        for g2 in range(14):
                            i = (ex * 2 + oh) * 14 + g2
                            s_ = i % 2
                            if i + 1 < n_d:
                                issue_d(i + 1)
                            for j in range(NJ):
                                for ci in range(2):
                                    c = g2 * 2 + ci
                                    S.op("pe", lambda e, j=j, ci=ci, c=c, s_=s_: e.matmul(psf[j][:], hT[:, c, j * 128:(j + 1) * 128], wd2[s_][:, ci, :], start=(c == 0), stop=(c == 27)),
                                         reads=[("hTs", c), ("wd2", s_)], writes=[("psf", j)])
                        for j in range(NJ):
                            S.op("act", lambda e, j=j, oh=oh: e.mul(out=Yv[:, j, oh * 512:(oh + 1) * 512], in_=psf[j][:], mul=gate_e[:, j:j + 1]),
                                 reads=[("psf", j), "gate_e"], writes=[("XY", j)])
                    for t in range(NT):
                        for half in range(2):
                            b_ = next6()
                            jmax = min(t, NJ - 1)
                            for j in range(jmax + 1):
                                S.op("pe", lambda e, j=j, t=t, half=half, b_=b_, jmax=jmax: e.matmul(psf[b_][:], GT[:, j, t * 128:(t + 1) * 128], Yv[:, j, half * 512:(half + 1) * 512], start=(j == 0), stop=(j == jmax)),
                                     reads=[("GT", j), ("XY", j)], writes=[("psf", b_)])
                            hs = h[:, t, half * 512:(half + 1) * 512]
                            S.op("dve", lambda e, hs=hs, b_=b_: e.tensor_tensor(out=hs, in0=psf[b_][:], in1=hs, op=ALU.add), reads=[("psf", b_), ("h", t)], writes=[("h", t)])
                S.flush()
            S2 = Sched(nc, es, "g")
            cur[0] = S2
            with ExitStack() as st3:
                xnT = sb("xnTg", [128, KC, T], BF16, st3)
                bufs = swiglu_bufs(st3, GS=2)
                for t in range(NT):
                    pb = next_psb()
                    for k in range(KC):
                        S.op("pe", lambda e, k=k, t=t, pb=pb: e.transpose(psb[pb][:, k * 128:(k + 1) * 128], xn_tok[:, t, k * 128:(k + 1) * 128], ident_b[:]),
                             reads=[("xn_tok", t), "ident_b"], writes=[("psb", pb)])
                    S.op("act", lambda e, t=t, pb=pb: e.copy(out=xnT[:, :, t * 128:(t + 1) * 128], in_=psb[pb][:].rearrange("p (k c) -> p k c", k=KC)),
                         reads=[("psb", pb)], writes=[("xnT", t)])
                for ex in range(8):
                    swiglu_block(xnT, "xnT", moe_wg[ex], moe_wu[ex], moe_wd[ex], 3584, st3,
                                 scale_col=lambda t, ex=ex: (comb_ov[:, t, ex:ex + 1], "comb_ov"), bufs=bufs)
                S2.flush(guard=flag_d[0:1, 0:1], outer=S_main)
            cur[0] = S_main

    def phase_out(do_norm=True):
        with ExitStack() as st:
            es_attn.close()
            ob = [sb("ob%d" % i, [128, D], F32, st) for i in range(2)]
            gbc_ref[0] = sb("gbc_o", [128, D], F32, st)
            if do_norm:
                load_gain(final_norm)
                norm_stats()
            for t in range(NT):
                b = t % 2
                if do_norm:
                    S.op("dve", lambda e, t=t, b=b: e.scalar_tensor_tensor(out=ob[b][:], in0=h[:, t, :], scalar=rstd[:, t:t + 1], in1=gbc_ref[0][:], op0=ALU.mult, op1=ALU.mult),
                         reads=[("h", t), ("rstd", t), "gbc"], writes=[("ob", b)])
                else:
                    S.op("dve", lambda e, t=t, b=b: e.tensor_copy(out=ob[b][:], in_=h[:, t, :]), reads=[("h", t)], writes=[("ob", b)])
                dma_sp(out_d[t * 128:(t + 1) * 128, :], ob[b][:], ("out", b), reads=[("ob", b)])
            S.flush()
            S.final_wait()

    phases = [("mla", phase_mla), ("ffn", phase_ffn), ("dsw", phase_dsw), ("moe", phase_moe)]
    stopped = False
    for name, fn in phases:
        if stop == "none":
            stopped = True
            break
        fn()
        if stop.startswith(name):
            stopped = True
            break
    phase_out(do_norm=not stopped)
    es_attn.close()
    es.close()
    return nc


_CACHE = {}


def kernel(**inputs):
    stop = STOP
    if stop not in _CACHE:
        _CACHE[stop] = build_program(stop)
    nc = _CACHE[stop]
    consts = host_consts()
    x = np.asarray(inputs["x"], dtype=np.float32)
    pos = np.asarray(inputs["positions"], dtype=np.int32)
    shared = {}
    for k in ("norm_attn", "norm_ffn", "dsw_w_kv"):
        shared[k] = np.ascontiguousarray(np.asarray(inputs[k], dtype=np.float32))
    for k in ("mla_w_down", "mla_w_uq", "mla_w_ukv", "mla_w_o", "dsw_w_q", "dsw_w_o", "ffn_w_gate", "ffn_w_up", "ffn_w_down",
              "moe_router", "moe_w_gate", "moe_w_up", "moe_w_down"):
        shared[k] = np.ascontiguousarray(np.asarray(inputs[k], dtype=np.float32)[0])
    shared["mla_q_norm"] = np.ascontiguousarray(np.asarray(inputs["mla_q_norm"], dtype=np.float32).reshape(1, 384))
    shared["mla_kv_norm"] = np.ascontiguousarray(np.asarray(inputs["mla_kv_norm"], dtype=np.float32).reshape(1, 256))
    shared["dsw_kv_norm"] = np.ascontiguousarray(np.asarray(inputs["dsw_kv_norm"], dtype=np.float32).reshape(1, D))
    shared["final_norm"] = np.ascontiguousarray(np.asarray(inputs["final_norm"], dtype=np.float32).reshape(1, D))
    shared.update(consts)
    ncores = int(os.environ.get("MK_CORES", "8"))
    in_maps = []
    for c in range(ncores):
        m = dict(shared)
        m["x"] = np.ascontiguousarray(x[c])
        m["pos"] = np.ascontiguousarray(pos[c].reshape(NT, 128).T)
        in_maps.append(m)
    res = run_bass_kernel_spmd(nc, in_maps, core_ids=list(range(ncores)))
    out = np.stack([np.asarray(r["out"], dtype=np.float32).reshape(T, D) for r in res.results], axis=0)
    if ncores < 8:
        out = np.concatenate([out, np.zeros((8 - ncores, T, D), np.float32)], axis=0)
    return out
```
